# Optimizing a Trainium2 kernel written in Bass

```python
import math
import jax, jax.numpy as jnp
from jax import lax
import numpy as np

D_MODEL = 1024
BATCH = 4
SEQ = 4096
DEPTH = 2

CHUNK = 64
N_META = 16
PAD_FRONT = CHUNK - N_META

SSD_EXPAND = 2
SSD_INNER = SSD_EXPAND * D_MODEL
SSD_HEAD_DIM = 64
SSD_HEADS = SSD_INNER // SSD_HEAD_DIM
SSD_GROUPS = 8
SSD_HPG = SSD_HEADS // SSD_GROUPS
SSD_STATE = 128
SSD_CONV = 4
SSD_GN = SSD_GROUPS * SSD_STATE
SSD_XBC = SSD_INNER + 2 * SSD_GN

POOL_WIDTH = D_MODEL
POOL_WINDOWS = (2, 4, 8, 16)
POOL_GROUPS = len(POOL_WINDOWS)
POOL_GROUP_DIM = POOL_WIDTH // POOL_GROUPS

N_BRANCH = 2
COL_Z = 0
COL_XBC = COL_Z + SSD_INNER
COL_DT = COL_XBC + SSD_XBC
COL_POOL = COL_DT + SSD_HEADS
COL_GATE = COL_POOL + POOL_WIDTH
IN_COLS = COL_GATE + N_BRANCH * D_MODEL

N_EXPERTS = 16
N_EXPERT_GROUPS = 4
EXPERTS_PER_GROUP = N_EXPERTS // N_EXPERT_GROUPS
TOP_K = 2
D_EXPERT = 512

DN_ALPHA = (2.0 * DEPTH) ** 0.25
DN_BETA = (8.0 * DEPTH) ** -0.25
LN_EPS = 1e-5
RMS_EPS = 1e-5

kernel_name = "hybrid_ssd_pool_moe_deepnorm"


def layer_norm(x, g, b):
    xf = x.astype(jnp.float32)
    mu = jnp.mean(xf, -1, keepdims=True)
    var = jnp.mean(jnp.square(xf - mu), -1, keepdims=True)
    return ((xf - mu) * lax.rsqrt(var + LN_EPS) * g + b).astype(x.dtype)


def causal_dwconv(x, w, b):
    y = lax.conv_general_dilated(
        x, w[:, None, :].astype(x.dtype), window_strides=(1,), padding=[(SSD_CONV - 1, 0)],
        dimension_numbers=("NWC", "WIO", "NWC"), feature_group_count=x.shape[-1])
    return y + b


def ssd_chunked(xs, dA, Bm, Cm):
    b, L = xs.shape[:2]

    def pad(t):
        return jnp.pad(t, [(0, 0), (PAD_FRONT, 0)] + [(0, 0)] * (t.ndim - 2))

    xs, dA, Bm, Cm = pad(xs), pad(dA), pad(Bm), pad(Cm)
    nc = (L + PAD_FRONT) // CHUNK
    xs = xs.reshape(b, nc, CHUNK, SSD_GROUPS, SSD_HPG, SSD_HEAD_DIM)
    Bm = Bm.reshape(b, nc, CHUNK, SSD_GROUPS, SSD_STATE)
    Cm = Cm.reshape(b, nc, CHUNK, SSD_GROUPS, SSD_STATE)
    a_cs = jnp.cumsum(dA.astype(jnp.float32).reshape(b, nc, CHUNK, SSD_GROUPS, SSD_HPG), axis=2)

    li = jnp.arange(CHUNK)
    causal = (li[:, None] >= li[None, :])[None, None, :, :, None, None]
    seg = a_cs[:, :, :, None] - a_cs[:, :, None, :]
    decay = jnp.exp(jnp.where(causal, seg, -jnp.inf))
    cb = jnp.einsum("bclgn,bcsgn->bclsg", Cm, Bm)
    y_diag = jnp.einsum("bclsg,bclsgr,bcsgrp->bclgrp", cb, decay, xs)

    decay_to_end = jnp.exp(a_cs[:, :, -1:] - a_cs)
    states = jnp.einsum("bclgn,bclgr,bclgrp->bcgrpn", Bm, decay_to_end, xs)
    chunk_decay = jnp.exp(a_cs[:, :, -1])

    def step(h, inp):
        s_c, d_c = inp
        return h * d_c[..., None, None] + s_c, h

    h0 = jnp.zeros((b,) + states.shape[2:], states.dtype)
    _, prev = lax.scan(step, h0, (jnp.moveaxis(states, 1, 0), jnp.moveaxis(chunk_decay, 1, 0)))
    prev = jnp.moveaxis(prev, 0, 1)
    y_off = jnp.einsum("bclgn,bcgrpn,bclgr->bclgrp", Cm, prev, jnp.exp(a_cs))

    y = (y_diag + y_off).reshape(b, nc * CHUNK, SSD_GROUPS, SSD_HPG, SSD_HEAD_DIM)
    return y[:, PAD_FRONT:]


def ssd_branch(proj, conv_w, conv_b, dt_bias, a_log, d_skip, norm_w):
    b, L, _ = proj.shape
    z = proj[..., COL_Z:COL_XBC]
    xbc = jax.nn.silu(causal_dwconv(proj[..., COL_XBC:COL_DT], conv_w, conv_b))
    xh = xbc[..., :SSD_INNER].reshape(b, L, SSD_GROUPS, SSD_HPG, SSD_HEAD_DIM)
    Bm = xbc[..., SSD_INNER:SSD_INNER + SSD_GN].reshape(b, L, SSD_GROUPS, SSD_STATE)
    Cm = xbc[..., SSD_INNER + SSD_GN:].reshape(b, L, SSD_GROUPS, SSD_STATE)
    dt = jax.nn.softplus(proj[..., COL_DT:COL_POOL].astype(jnp.float32) + dt_bias)
    dt = dt.reshape(b, L, SSD_GROUPS, SSD_HPG)
    A = -jnp.exp(a_log.astype(jnp.float32)).reshape(SSD_GROUPS, SSD_HPG)
    y = ssd_chunked(xh * dt[..., None], dt * A, Bm, Cm)
    y = y + d_skip.reshape(SSD_GROUPS, SSD_HPG)[:, :, None] * xh
    y = y.reshape(b, L, SSD_INNER)
    yg = (y * jax.nn.silu(z.astype(jnp.float32))).reshape(b, L, SSD_GROUPS, -1)
    yg = yg * lax.rsqrt(jnp.mean(jnp.square(yg), -1, keepdims=True) + RMS_EPS)
    return (yg.reshape(b, L, SSD_INNER) * norm_w).astype(proj.dtype)


def pool_branch(p, w_group, scale):
    b, L, _ = p.shape
    pf = p.astype(jnp.float32).reshape(b, L, POOL_GROUPS, POOL_GROUP_DIM)
    cs = jnp.pad(jnp.cumsum(pf, axis=1), [(0, 0), (1, 0), (0, 0), (0, 0)])
    t = jnp.arange(L)[:, None]
    win = jnp.array(POOL_WINDOWS, dtype=jnp.int32)[None, :]
    start = jnp.maximum(t + 1 - win, 0)
    gidx = jnp.arange(POOL_GROUPS)[None, :]
    wsum = cs[:, 1:] - cs[:, start, gidx]
    cnt = jnp.minimum(t + 1, win).astype(jnp.float32)
    mixed = wsum / cnt[None, :, :, None] - pf
    out = jnp.einsum("blgc,gcd->blgd", mixed, w_group).reshape(b, L, POOL_WIDTH)
    return (out * scale).astype(p.dtype)


def moe_ffn(x, w_router, w_gate, w_up, w_down):
    b, L, d = x.shape
    t = x.reshape(-1, d)
    scores = jax.nn.softmax((t @ w_router).astype(jnp.float32), axis=-1)
    grp = scores.reshape(-1, N_EXPERT_GROUPS, EXPERTS_PER_GROUP)
    grp_score = lax.top_k(grp, TOP_K)[0].sum(-1)
    sel = jnp.argmax(grp_score, axis=-1)
    in_grp = (jnp.arange(N_EXPERTS) // EXPERTS_PER_GROUP)[None, :] == sel[:, None]
    topv, topi = lax.top_k(jnp.where(in_grp, scores, -1.0), TOP_K)
    topv = topv / jnp.sum(topv, -1, keepdims=True)
    comb = jnp.sum(jax.nn.one_hot(topi, N_EXPERTS, dtype=jnp.float32) * topv[..., None], axis=1)
    out = jnp.zeros(t.shape, jnp.float32)
    for e in range(N_EXPERTS):
        h = jax.nn.silu(t @ w_gate[e]) * (t @ w_up[e])
        out = out + comb[:, e:e + 1] * (h @ w_down[e])
    return out.astype(x.dtype).reshape(b, L, d)


def setup_inputs(seed: int = 0) -> dict:
    key = jax.random.key(seed)
    ks = jax.random.split(key, 24)
    f32 = jnp.float32

    def nrm(k, shape, scale):
        return jax.random.normal(k, shape, f32) * scale

    dt = jnp.exp(jax.random.uniform(ks[6], (DEPTH, SSD_HEADS), f32)
                 * (math.log(0.1) - math.log(1e-3)) + math.log(1e-3))
    return {
        "x": nrm(ks[0], (BATCH, SEQ, D_MODEL), 1.0),
        "meta_tokens": nrm(ks[1], (N_META, D_MODEL), 1.0),
        "ln_in_g": 1.0 + nrm(ks[2], (D_MODEL,), 0.02),
        "ln_in_b": nrm(ks[3], (D_MODEL,), 0.02),
        "w_router": nrm(ks[4], (D_MODEL, N_EXPERTS), D_MODEL ** -0.5),
        "w_in": nrm(ks[5], (DEPTH, D_MODEL, IN_COLS), D_MODEL ** -0.5),
        "conv_w": nrm(ks[7], (DEPTH, SSD_CONV, SSD_XBC), SSD_CONV ** -0.5),
        "conv_b": nrm(ks[8], (DEPTH, SSD_XBC), 0.02),
        "dt_bias": dt + jnp.log(-jnp.expm1(-dt)),
        "a_log": jnp.log(jax.random.uniform(ks[9], (DEPTH, SSD_HEADS), f32, 1.0, 16.0)),
        "d_skip": 1.0 + nrm(ks[10], (DEPTH, SSD_HEADS), 0.02),
        "ssd_norm_w": 1.0 + nrm(ks[11], (DEPTH, SSD_INNER), 0.02),
        "w_ssd_out": nrm(ks[12], (DEPTH, SSD_INNER, D_MODEL), SSD_INNER ** -0.5 * DN_BETA),
        "w_pool": nrm(ks[13], (DEPTH, POOL_GROUPS, POOL_GROUP_DIM, POOL_GROUP_DIM), POOL_GROUP_DIM ** -0.5 * DN_BETA),
        "pool_scale": 1.0 + nrm(ks[14], (DEPTH, POOL_WIDTH), 0.02),
        "b_gate": nrm(ks[15], (DEPTH, N_BRANCH * D_MODEL), 0.02),
        "w_out": nrm(ks[16], (DEPTH, D_MODEL, D_MODEL), D_MODEL ** -0.5 * DN_BETA),
        "ln1_g": 1.0 + nrm(ks[17], (DEPTH, D_MODEL), 0.02),
        "ln1_b": nrm(ks[18], (DEPTH, D_MODEL), 0.02),
        "w_exp_gate": nrm(ks[19], (DEPTH, N_EXPERTS, D_MODEL, D_EXPERT), D_MODEL ** -0.5),
        "w_exp_up": nrm(ks[20], (DEPTH, N_EXPERTS, D_MODEL, D_EXPERT), D_MODEL ** -0.5 * DN_BETA),
        "w_exp_down": nrm(ks[21], (DEPTH, N_EXPERTS, D_EXPERT, D_MODEL), D_EXPERT ** -0.5 * DN_BETA),
        "ln2_g": 1.0 + nrm(ks[22], (DEPTH, D_MODEL), 0.02),
        "ln2_b": nrm(ks[23], (DEPTH, D_MODEL), 0.02),
    }


def reference(x, meta_tokens, ln_in_g, ln_in_b, w_router, w_in, conv_w, conv_b, dt_bias, a_log,
              d_skip, ssd_norm_w, w_ssd_out, w_pool, pool_scale, b_gate, w_out, ln1_g, ln1_b,
              w_exp_gate, w_exp_up, w_exp_down, ln2_g, ln2_b):
    b = x.shape[0]
    meta = jnp.broadcast_to(meta_tokens[None].astype(x.dtype), (b, N_META, D_MODEL))
    h = layer_norm(jnp.concatenate([meta, x], axis=1), ln_in_g, ln_in_b)
    L = h.shape[1]
    for i in range(DEPTH):
        proj = h @ w_in[i]
        y_ssd = ssd_branch(proj, conv_w[i], conv_b[i], dt_bias[i], a_log[i], d_skip[i],
                           ssd_norm_w[i]) @ w_ssd_out[i]
        y_pool = pool_branch(proj[..., COL_POOL:COL_GATE], w_pool[i], pool_scale[i])
        gates = jax.nn.sigmoid(proj[..., COL_GATE:] + b_gate[i]).reshape(b, L, N_BRANCH, D_MODEL)
        mixed = (gates[:, :, 0] * y_ssd + gates[:, :, 1] * y_pool) @ w_out[i]
        h = layer_norm(DN_ALPHA * h + mixed, ln1_g[i], ln1_b[i])
        ffn = moe_ffn(h, w_router, w_exp_gate[i], w_exp_up[i], w_exp_down[i])
        h = layer_norm(DN_ALPHA * h + ffn, ln2_g[i], ln2_b[i])
    return h[:, N_META:]
```

```python
import numpy as np
import concourse.bass as bass
import concourse.mybir as mybir
from concourse.bass_utils import run_bass_kernel_spmd

F32 = mybir.dt.float32
BF16 = mybir.dt.bfloat16
U8 = mybir.dt.uint8
AF = mybir.ActivationFunctionType
ALU = mybir.AluOpType
AX = mybir.AxisListType

D = 1024
NSLOT = 4160
CH = 64
TT = 256
NH = 32
NG = 8
COL_Z, COL_X, COL_B, COL_C, COL_DT, COL_POOL, COL_GATE, INC = 0, 2048, 4096, 5120, 6144, 6176, 7200, 9248
NE = 16
ALPHA = 4.0 ** 0.25
LN_EPS = 1e-5
RMS_EPS = 1e-5
NO_SELF_SYNC = False
NW = 4
ARENA = 55296
POOLW = (2, 4, 8, 16)


class Tok:
    __slots__ = ("w", "r", "name")

    def __init__(self, name=""):
        self.w = None
        self.r = {}
        self.name = name


class _Eng:
    def __init__(self, name, e, sem):
        self.name, self.e, self.sem, self.cnt, self.known = name, e, sem, 0, {}


class _DSem:
    def __init__(self, h):
        self.h, self.cnt = h, 0


class Sched:
    def __init__(self, nc):
        self.nc = nc
        self.nsem = 0
        self.engs = {}
        for name, e in (("pe", nc.tensor), ("act", nc.scalar), ("dve", nc.vector), ("pool", nc.gpsimd),
                        ("sp", nc.sync)):
            self.engs[name] = _Eng(name, e, self._sem("s_" + name))

    def _sem(self, name):
        self.nsem += 1
        return self.nc.semaphore(name).__enter__()

    def dsem(self, name):
        return _DSem(self._sem("d_" + name))

    def _wait(self, E, deps):
        best = {}
        for sem, val in deps:
            k = id(sem)
            if k not in best or best[k][1] < val:
                best[k] = (sem, val)
        for k, (sem, val) in best.items():
            if sem is E.sem and (E.name == "pe" or NO_SELF_SYNC):
                continue
            if E.known.get(k, 0) >= val:
                continue
            E.e.wait_ge(sem, val)
            E.known[k] = val

    @staticmethod
    def _deps(reads, writes):
        deps = []
        for t in reads:
            if t.w is not None:
                deps.append(t.w)
        for t in writes:
            if t.w is not None:
                deps.append(t.w)
            deps.extend(t.r.values())
        return deps

    def op(self, eng, fn, reads=(), writes=()):
        E = self.engs[eng]
        self._wait(E, self._deps(reads, writes))
        ins = fn(E.e)
        E.cnt += 1
        ins.then_inc(E.sem, 1)
        mark = (E.sem, E.cnt)
        for t in writes:
            t.w = mark
            t.r = {}
        for t in reads:
            if t.w is not mark:
                t.r[id(E.sem)] = mark

    def dma(self, queue, out, in_, ds, reads=(), writes=(), **kw):
        Q = self.engs[queue]
        self._wait(Q, self._deps(reads, writes))
        ins = Q.e.dma_start(out=out, in_=in_, **kw)
        ds.cnt += 16
        ins.then_inc(ds.h, 16)
        mark = (ds.h, ds.cnt)
        for t in writes:
            t.w = mark
            t.r = {}
        for t in reads:
            t.r[id(ds.h)] = mark

    def barrier(self, names=("pe", "act", "dve", "pool")):
        for a in names:
            A = self.engs[a]
            deps = [(self.engs[b].sem, self.engs[b].cnt) for b in names if b != a and self.engs[b].cnt > 0]
            self._wait(A, deps)

    def wait_all(self, eng, dsems):
        E = self.engs[eng]
        deps = [(x.sem, x.cnt) for x in self.engs.values() if x is not E and x.cnt > 0]
        deps += [(d.h, d.cnt) for d in dsems if d.cnt > 0]
        self._wait(E, deps)


def bc(ap, axis, shape):
    return ap.unsqueeze(axis).broadcast_to(list(shape))


def seq_tiles():
    tiles = [(0, 64)] + [(64 + i * TT, TT) for i in range((NSLOT - 64) // TT)]
    blocks = [[0, 1, 2, 3, 4]] + [list(range(5 + 4 * i, 9 + 4 * i)) for i in range(3)]
    return tiles, blocks


class _Stop(Exception):
    pass


def build(nblocks=4, nlayers=2, dbg=False, upto="full", ncores=8, trace=False):
    nc = bass.Bass("TRN2", target_bir_lowering=False)
    S = Sched(nc)

    def dram(name, shape, dt=F32, kind="ExternalInput"):
        return nc.dram_tensor(name, list(shape), dt, kind=kind).ap()

    xin = dram("xin", [NSLOT, D])
    w_in = dram("w_in", [2, D, INC])
    w_so = dram("w_so", [2, 2048, D])
    w_pool = dram("w_pool", [2, 4, 256, 256])
    w_out = dram("w_out", [2, D, D])
    w_eg = dram("w_eg", [2, NE, D, 512])
    w_eu = dram("w_eu", [2, NE, D, 512])
    w_ed = dram("w_ed", [2, NE, 512, D])
    lnt_d = dram("lnt", [10, 128, D])
    cst_d = dram("cst", [128, 1024])
    convw_d = dram("convw", [2, 128, 32, 5])
    vec_d = dram("vecs", [2, 128, 64])
    wr_d = dram("wr", [128, 8, 16])
    out_d = dram("out", [4096, D], kind="ExternalOutput")
    dbg_d = dram("dbg", [128, 8192], kind="ExternalOutput") if dbg else None
    dbgb_d = dram("dbgb", [128, 8192], BF16, kind="ExternalOutput") if dbg else None

    def sb(name, shape, dt=F32):
        return nc.alloc_sbuf_tensor(name, list(shape), dt)

    hres = sb("hres", [128, 9, D])
    hT = sb("hT", [128, 8, 1088], BF16)
    xT = sb("xT", [128, 16, TT], BF16)
    yT = sb("yT", [128, 16, TT], BF16)
    st32 = [sb(f"st32_{l}", [128, 2048]) for l in range(2)]
    stb = [sb(f"stb_{l}", [128, 2048], BF16) for l in range(2)]
    chist = [sb(f"chist_{l}", [128, 32, 3]) for l in range(2)]
    phist = [sb(f"phist_{l}", [128, 8, 15]) for l in range(2)]
    lnt = sb("lnt_sb", [128, 2, D])
    ring = [sb(f"ring{i}", [128, 4096], BF16) for i in range(NW)]
    cst = sb("cst_sb", [128, 1024])
    convw = sb("convw_sb", [128, 2, 32, 5])
    vecs = sb("vecs_sb", [128, 2, 64])
    wr = sb("wr_sb", [128, 8, 16])
    comb = sb("comb", [128, 9, 16])
    identb = sb("identb", [128, 128], BF16)
    onesb = sb("onesb", [128, 128], BF16)
    negmb = sb("negmb", [64, 64], BF16)
    Aneg = sb("Aneg", [32, 2])
    wrh = sb("wrh", [128, 8, 16], BF16)
    wrl = sb("wrl", [128, 8, 16], BF16)
    wpool_sb = sb("wpool_sb", [128, 2, 2048], BF16)
    arena = sb("arena", [128, ARENA], U8)

    T_hres = [Tok(f"hres{j}") for j in range(9)]
    T_hT = [Tok(f"hT{j}") for j in range(9)]
    T_xT, T_yT = Tok("xT"), Tok("yT")
    T_st32 = [Tok(), Tok()]
    T_stb = [Tok(), Tok()]
    T_chist = [Tok(), Tok()]
    T_phist = [Tok(), Tok()]
    T_lnt = [Tok(), Tok()]
    T_ring = [Tok(f"ring{i}") for i in range(NW)]
    T_cst = Tok("cst")
    T_comb = [Tok() for _ in range(9)]
    T_out = Tok("out")

    D_ring = [S.dsem(f"ring{i}") for i in range(NW)]
    D_in = S.dsem("in")
    D_x = [S.dsem(f"x{j}") for j in range(9)]
    D_lnt = [S.dsem("lnt0"), S.dsem("lnt1")]
    D_out = S.dsem("out")
    D_row = S.dsem("arow")
    D_dbg = S.dsem("dbg")

    ident32 = cst[:, 0:128]
    ones32 = cst[:, 128:256]
    tri32 = cst[0:64, 256:320]
    negm32 = cst[0:64, 320:384]
    rc0 = cst[:, 384:640].rearrange("p (g l) -> p g l", g=4)
    m0 = cst[0:64, 640:641]
    epsc = cst[:, 641:644]

    psum = [nc.alloc_psum_tensor(f"ps{i}", [128, 512], F32) for i in range(8)]
    T_ps = [Tok(f"ps{i}") for i in range(8)]
    ps_rr = [0]

    def PS():
        i = ps_rr[0]
        ps_rr[0] = (i + 1) % 8
        return psum[i], T_ps[i]

    aoff = [0]
    a_live = []
    a_old = []

    def A_reset(barrier=False):
        if barrier:
            S.barrier()
        a_old.extend(a_live)
        del a_live[:]
        aoff[0] = 0

    def A(shape, dt=F32, tok=None):
        esz = {F32: 4, BF16: 2}[dt]
        n = 1
        for s in shape[1:]:
            n *= s
        nb = (n * esz + 31) // 32 * 32
        o = aoff[0]
        aoff[0] += nb
        assert aoff[0] <= ARENA, f"arena overflow {aoff[0]}"
        v = arena[0:shape[0], o:o + n * esz].bitcast(dt)
        if len(shape) == 3:
            v = v.rearrange("p (a b) -> p a b", a=shape[1])
        elif len(shape) == 4:
            v = v.rearrange("p (a b c) -> p a b c", a=shape[1], b=shape[2])
        tk = tok if tok is not None else Tok()
        keep = []
        for (s0, e0, t0) in a_old:
            if e0 <= o or s0 >= o + nb:
                keep.append((s0, e0, t0))
                continue
            marks = list(t0.r.values())
            if t0.w is not None:
                marks.append(t0.w)
            for (sem, val) in marks:
                k = id(sem)
                if k not in tk.r or tk.r[k][1] < val:
                    tk.r[k] = (sem, val)
            if not (s0 >= o and e0 <= o + nb):
                keep.append((s0, e0, t0))
        a_old[:] = keep
        a_live.append((o, o + nb, tk))
        return v, tk

    dbg_col = [0]

    def dump(ap, tok, rows, ncols):
        if not dbg:
            return
        c = dbg_col[0]
        dbg_col[0] += ncols
        S.dma("sp", dbg_d[0:rows, c:c + ncols], ap, D_dbg, reads=[tok])
        return c

    dbgb_col = [0]

    def dump16(ap, tok, rows, ncols):
        if not dbg:
            return
        c = dbgb_col[0]
        dbgb_col[0] += ncols
        S.dma("sp", dbgb_d[0:rows, c:c + ncols], ap, D_dbg, reads=[tok])

    ring_i = [0]

    def wload(parts):
        i = ring_i[0]
        ring_i[0] = (i + 1) % NW
        for dst_fn, src in parts:
            S.dma("pool", dst_fn(ring[i]), src, D_ring[i], writes=[T_ring[i]])
        return ring[i], T_ring[i]

    NPC = 24
    wsc = dram("wsc", [2 * NPC, 128, 4096], BF16, kind="Internal")
    T_wsc = [Tok("wsc0"), Tok("wsc1")]
    D_wst = S.dsem("wst")
    wsc_ready = [False, False]

    def wpiece(l, idx, parts):
        if not wsc_ready[l]:
            pc, tk = wload(parts)
            S.dma("sp", wsc[l * NPC + idx], pc[:, :], D_wst, reads=[tk])
            T_wsc[l].w = (D_wst.h, D_wst.cnt)
            return pc, tk
        i = ring_i[0]
        ring_i[0] = (i + 1) % NW
        S.dma("pool", ring[i][:, :], wsc[l * NPC + idx], D_ring[i], reads=[T_wsc[l]], writes=[T_ring[i]])
        return ring[i], T_ring[i]

    def w_in_piece(l, c0, ncol, idx=None):
        src = w_in[l].rearrange("(kc p) c -> p kc c", p=128)[:, :, c0:c0 + ncol]
        parts = [(lambda r: r[:, 0:8 * ncol].rearrange("p (kc c) -> p kc c", kc=8), src)]
        pc, tk = wload(parts) if idx is None else wpiece(l, idx, parts)
        return pc[:, 0:8 * ncol].rearrange("p (kc c) -> p kc c", kc=8), tk

    S.dma("sp", cst[:], cst_d[:, :], D_in, writes=[T_cst])
    S.dma("sp", convw[:], convw_d.rearrange("l p b j -> p l b j"), D_in, writes=[T_cst])
    S.dma("sp", vecs[:], vec_d.rearrange("l p c -> p l c"), D_in, writes=[T_cst])
    S.dma("sp", wr[:], wr_d[:, :, :], D_in, writes=[T_cst])
    S.op("act", lambda e: e.activation(out=identb[:], in_=ident32, func=AF.Copy), reads=[T_cst], writes=[T_cst])
    S.op("act", lambda e: e.activation(out=onesb[:], in_=ones32, func=AF.Copy), reads=[T_cst], writes=[T_cst])
    S.op("act", lambda e: e.activation(out=negmb[:], in_=negm32, func=AF.Copy), reads=[T_cst], writes=[T_cst])
    S.op("act", lambda e: e.activation(out=wrh[:], in_=wr[:], func=AF.Copy), reads=[T_cst], writes=[T_cst])
    S.op("dve", lambda e: e.tensor_tensor(out=wrl[:], in0=wr[:], in1=wrh[:], op=ALU.subtract), reads=[T_cst], writes=[T_cst])
    S.op("act", lambda e: e.activation(out=Aneg[:, 0:1], in_=vecs[0:32, 0, 57:58], func=AF.Exp),
         reads=[T_cst], writes=[T_cst])
    S.op("act", lambda e: e.activation(out=Aneg[:, 1:2], in_=vecs[0:32, 1, 57:58], func=AF.Exp),
         reads=[T_cst], writes=[T_cst])
    S.op("dve", lambda e: e.tensor_scalar(out=Aneg[:], in0=Aneg[:], scalar1=-1.0, scalar2=None, op0=ALU.mult),
         reads=[T_cst], writes=[T_cst])
    D_wp = S.dsem("wpool")
    for l in range(2):
        S.dma("pool", wpool_sb[:, l, :].rearrange("p (g c d) -> p g c d", g=4, c=2),
              w_pool[l].rearrange("g (c p) d -> p g c d", p=128), D_wp, writes=[T_cst])
    for l in range(2):
        S.op("dve", lambda e, l=l: e.memset(st32[l][:], 0.0), writes=[T_st32[l]])
        S.op("dve", lambda e, l=l: e.memset(stb[l][:], 0.0), writes=[T_stb[l]])
        S.op("dve", lambda e, l=l: e.memset(chist[l][:], 0.0), writes=[T_chist[l]])
        S.op("dve", lambda e, l=l: e.memset(phist[l][:], 0.0), writes=[T_phist[l]])

    tiles, blocks = seq_tiles()

    def load_lnt(slot, idx):
        S.dma("sp", lnt[:, slot, :], lnt_d[idx], D_lnt[slot], writes=[T_lnt[slot]])

    def layer_norm_inplace(j, rows, pre_fn=None):
        h = hres[0:rows, j, :]
        tk = T_hres[j]
        stats, t_s = A([128, 2, 6])
        mv, _ = A([128, 2], tok=t_s)
        rstd, _ = A([128, 1], tok=t_s)
        nb, _ = A([128, 1], tok=t_s)
        for c in range(2):
            S.op("dve", lambda e, c=c: e.bn_stats(out=stats[0:rows, c, :], in_=h[:, c * 512:(c + 1) * 512]),
                 reads=[tk], writes=[t_s])
        S.op("dve", lambda e: e.bn_aggr(out=mv[0:rows, :], in_=stats[0:rows, :, :].rearrange("p a b -> p (a b)")), reads=[t_s], writes=[t_s])
        S.op("act", lambda e: e.activation(out=rstd[0:rows, :], in_=mv[0:rows, 1:2], func=AF.Ln, bias=epsc[0:rows, 0:1], scale=1.0),
             reads=[t_s, T_cst], writes=[t_s])
        S.op("act", lambda e: e.activation(out=rstd[0:rows, :], in_=rstd[0:rows, :], func=AF.Exp, scale=-0.5),
             reads=[t_s], writes=[t_s])
        S.op("dve", lambda e: e.scalar_tensor_tensor(out=nb[0:rows, :], in0=mv[0:rows, 0:1], scalar=-1.0,
                                                     in1=rstd[0:rows, :], op0=ALU.mult, op1=ALU.mult),
             reads=[t_s], writes=[t_s])
        S.op("act", lambda e: e.activation(out=h, in_=h, func=AF.Identity, bias=nb[0:rows, :], scale=rstd[0:rows, :]),
             reads=[tk, t_s], writes=[tk])
        S.op("dve", lambda e: e.tensor_tensor(out=h, in0=h, in1=lnt[0:rows, 0, :], op=ALU.mult),
             reads=[tk, T_lnt[0]], writes=[tk])
        S.op("dve", lambda e: e.tensor_tensor(out=h, in0=h, in1=lnt[0:rows, 1, :], op=ALU.add),
             reads=[tk, T_lnt[1]], writes=[tk])

    def transpose_to_hT(j, rows, btok, router, mask0):
        tk = T_hres[j]
        if mask0:
            S.op("dve", lambda e: e.tensor_scalar(out=hres[0:rows, j, :], in0=hres[0:rows, j, :], scalar1=m0,
                                                  scalar2=None, op0=ALU.mult), reads=[tk, T_cst], writes=[tk])
        hb, t_hb = A([128, D], BF16)
        S.op("act", lambda e: e.activation(out=hb[0:rows, :], in_=hres[0:rows, j, :], func=AF.Copy), reads=[tk], writes=[t_hb])
        ps, tp = PS()
        psb = ps[:, :].bitcast(BF16)
        for kc in range(8):
            S.op("pe", lambda e, kc=kc, psb=psb: e.transpose(out=psb[:, kc * 128:kc * 128 + rows],
                                                             in_=hb[0:rows, kc * 128:(kc + 1) * 128],
                                                             identity=identb[0:rows, 0:rows]),
                 reads=[t_hb, T_cst], writes=[tp])
        S.op("act", lambda e, psb=psb: e.activation(out=hT[:, :, btok:btok + rows],
                                                    in_=psb[:, :].rearrange("p (a b) -> p a b", a=8)[:, :, 0:rows], func=AF.Copy),
             reads=[tp], writes=[T_hT[j]])
        if not router:
            return
        hlo, t_hlo = A([128, D], BF16)
        hTlo, t_hTlo = A([128, 8, 128], BF16)
        S.op("dve", lambda e: e.tensor_tensor(out=hlo[0:rows, :], in0=hres[0:rows, j, :], in1=hb[0:rows, :], op=ALU.subtract),
             reads=[tk, t_hb], writes=[t_hlo])
        ps, tp = PS()
        psb = ps[:, :].bitcast(BF16)
        for kc in range(8):
            S.op("pe", lambda e, kc=kc, psb=psb: e.transpose(out=psb[:, kc * 128:kc * 128 + rows],
                                                             in_=hlo[0:rows, kc * 128:(kc + 1) * 128],
                                                             identity=identb[0:rows, 0:rows]),
                 reads=[t_hlo, T_cst], writes=[tp])
        S.op("dve", lambda e, psb=psb: e.tensor_copy(out=hTlo[:, :, 0:rows],
                                                     in_=psb[:, :].rearrange("p (a b) -> p a b", a=8)[:, :, 0:rows]),
             reads=[tp], writes=[t_hTlo])
        ps, tp = PS()
        n = 0
        for kc in range(8):
            for (lt, rd, rh) in ((hT[:, kc, btok:btok + rows], T_hT[j], wrh[:, kc, :]),
                                 (hT[:, kc, btok:btok + rows], T_hT[j], wrl[:, kc, :]),
                                 (hTlo[:, kc, 0:rows], t_hTlo, wrh[:, kc, :])):
                S.op("pe", lambda e, lt=lt, rh=rh, n=n, ps=ps: e.matmul(ps[0:rows, 0:16], lhsT=lt, rhs=rh, start=(n == 0), stop=(n == 23)),
                     reads=[rd, T_cst], writes=[tp])
                n += 1
        R = slice(0, rows)
        mx, t_k = A([128, 1])
        ee, _ = A([128, 4, 4], tok=t_k)
        m1, _ = A([128, 4], tok=t_k)
        eq1, _ = A([128, 4, 4], tok=t_k)
        e2, _ = A([128, 4, 4], tok=t_k)
        m2, _ = A([128, 4], tok=t_k)
        eq2, _ = A([128, 4, 4], tok=t_k)
        gs, _ = A([128, 4], tok=t_k)
        gm, _ = A([128, 1], tok=t_k)
        sr, _ = A([128, 4], tok=t_k)
        w1, _ = A([128, 4], tok=t_k)
        w2, _ = A([128, 4], tok=t_k)
        cb = comb[R, j, :].rearrange("p (g e) -> p g e", g=4)
        tc_ = T_comb[j]
        V = lambda fn, rd=(), wr_=(): S.op("dve", fn, reads=[t_k] + list(rd), writes=[t_k] + list(wr_))
        V(lambda e: e.tensor_reduce(out=mx[R, :], in_=ps[R, 0:16], axis=AX.X, op=ALU.max), rd=[tp])
        V(lambda e: e.tensor_scalar(out=mx[R, :], in0=mx[R, :], scalar1=-1.0, scalar2=None, op0=ALU.mult))
        S.op("act", lambda e: e.activation(out=ee[R].rearrange("p g e -> p (g e)"), in_=ps[R, 0:16], func=AF.Exp,
                                           bias=mx[R, :], scale=1.0), reads=[tp, t_k], writes=[t_k])
        V(lambda e: e.tensor_reduce(out=m1[R, :], in_=ee[R], axis=AX.X, op=ALU.max))
        V(lambda e: e.tensor_tensor(out=eq1[R], in0=ee[R], in1=bc(m1[R, :], 2, [rows, 4, 4]),
                                    op=ALU.is_equal))
        V(lambda e: e.scalar_tensor_tensor(out=e2[R], in0=eq1[R], scalar=-4.0, in1=ee[R], op0=ALU.mult, op1=ALU.add))
        V(lambda e: e.tensor_reduce(out=m2[R, :], in_=e2[R], axis=AX.X, op=ALU.max))
        V(lambda e: e.tensor_tensor(out=eq2[R], in0=e2[R], in1=bc(m2[R, :], 2, [rows, 4, 4]),
                                    op=ALU.is_equal))
        V(lambda e: e.tensor_tensor(out=gs[R, :], in0=m1[R, :], in1=m2[R, :], op=ALU.add))
        V(lambda e: e.tensor_reduce(out=gm[R, :], in_=gs[R, :], axis=AX.X, op=ALU.max))
        V(lambda e: e.tensor_scalar(out=sr[R, :], in0=gs[R, :], scalar1=gm[R, :], scalar2=None, op0=ALU.is_equal))
        V(lambda e: e.reciprocal(out=gm[R, :], in_=gm[R, :]))
        V(lambda e: e.tensor_scalar(out=sr[R, :], in0=sr[R, :], scalar1=gm[R, :], scalar2=None, op0=ALU.mult))
        V(lambda e: e.tensor_tensor(out=w1[R, :], in0=m1[R, :], in1=sr[R, :], op=ALU.mult))
        V(lambda e: e.tensor_tensor(out=w2[R, :], in0=m2[R, :], in1=sr[R, :], op=ALU.mult))
        V(lambda e: e.tensor_tensor(out=eq1[R], in0=eq1[R], in1=bc(w1[R, :], 2, [rows, 4, 4]), op=ALU.mult))
        V(lambda e: e.tensor_tensor(out=eq2[R], in0=eq2[R], in1=bc(w2[R, :], 2, [rows, 4, 4]), op=ALU.mult))
        V(lambda e: e.tensor_tensor(out=cb, in0=eq1[R], in1=eq2[R], op=ALU.add), wr_=[tc_])

    def mixer(l, ti, subs, btok0, first_tile):
        slot0, T = tiles[ti]
        nch = T // CH
        tsl = slice(btok0, btok0 + T)
        T_hTt = [T_hT[j] for j, _, _ in subs]
        cw = convw[:, l]

        A_reset()
        BT, t_BT = A([128, 8, TT], BF16)
        CT, t_CT = A([128, 8, TT], BF16)
        pre = [A([128, 3 + TT]) for _ in range(3)]
        cacc = [A([128, TT]) for _ in range(3)]
        wv_dt, tw_dt = w_in_piece(l, COL_DT, 32)
        dtT, t_dt = A([32, TT])
        dAT, _ = A([32, TT], tok=t_dt)
        acsT, _ = A([32, TT], tok=t_dt)
        sdtT, _ = A([32, TT], tok=t_dt)
        tmpT, _ = A([32, TT], tok=t_dt)
        onesT, _ = A([32, CH], tok=t_dt)
        hiT, t_hl = A([32, TT], BF16)
        loT, _ = A([32, TT], BF16, tok=t_hl)

        def dt_chain():
            ps, tp = PS()
            for kc in range(8):
                S.op("pe", lambda e, kc=kc: e.matmul(ps[0:32, 0:T], lhsT=wv_dt[:, kc, 0:32], rhs=hT[:, kc, tsl],
                                                     start=(kc == 0), stop=(kc == 7)), reads=[tw_dt] + T_hTt, writes=[tp])
            yield
            S.op("act", lambda e: e.activation(out=tmpT[:, 0:T], in_=ps[0:32, 0:T], func=AF.Exp,
                                               bias=vecs[0:32, l, 56:57], scale=1.0), reads=[tp, T_cst], writes=[t_dt])
            S.op("act", lambda e: e.activation(out=dtT[:, 0:T], in_=tmpT[:, 0:T], func=AF.Ln, bias=epsc[0:32, 2:3], scale=1.0),
                 reads=[t_dt], writes=[t_dt])
            yield
            if ti == 0:
                S.op("dve", lambda e: e.memset(dtT[:, 0:48], 0.0), reads=[t_dt], writes=[t_dt])
            S.op("dve", lambda e: e.tensor_scalar(out=dAT[:, 0:T], in0=dtT[:, 0:T], scalar1=Aneg[:, l:l + 1], scalar2=None,
                                                  op0=ALU.mult), reads=[t_dt, T_cst], writes=[t_dt])
            S.op("dve", lambda e: e.memset(onesT[:], 1.0), writes=[t_dt])
            for c in range(nch):
                cs = slice(c * CH, (c + 1) * CH)
                S.op("dve", lambda e, cs=cs: e.tensor_tensor_scan(out=acsT[:, cs], data0=onesT[:], data1=dAT[:, cs],
                                                                  initial=0.0, op0=ALU.mult, op1=ALU.add),
                     reads=[t_dt], writes=[t_dt])
            yield
            for c in range(nch):
                cs = slice(c * CH, (c + 1) * CH)
                S.op("act", lambda e, cs=cs, c=c: e.activation(out=tmpT[:, cs], in_=acsT[:, cs], func=AF.Exp,
                                                               bias=acsT[:, c * CH + 63:c * CH + 64], scale=-1.0),
                     reads=[t_dt], writes=[t_dt])
            S.op("act", lambda e: e.activation(out=hiT[:, 0:T], in_=acsT[:, 0:T], func=AF.Copy), reads=[t_dt], writes=[t_hl])
            yield
            S.op("dve", lambda e: e.tensor_tensor(out=sdtT[:, 0:T], in0=dtT[:, 0:T], in1=tmpT[:, 0:T], op=ALU.mult),
                 reads=[t_dt], writes=[t_dt])
            S.op("dve", lambda e: e.tensor_tensor(out=loT[:, 0:T], in0=acsT[:, 0:T], in1=hiT[:, 0:T], op=ALU.subtract),
                 reads=[t_dt, t_hl], writes=[t_hl])
            yield

        dtg = dt_chain()
        dt_steps = {1: 1, 5: 1, 9: 1, 13: 1, 17: 1} if T == TT else {}
        def hist_in(b):
            pbn, t_pbn = pre[b % 3]
            S.op("dve", lambda e: e.tensor_copy(out=pbn[:, 0:3], in_=chist[l][:, b, :]),
                 reads=[T_chist[l]], writes=[t_pbn])

        hist_in(0)
        pend_silu = None
        for pi in range(8):
            wv, tw = w_in_piece(l, COL_X + pi * 512, 512, idx=pi)
            for cbk in range(4):
                b = pi * 4 + cbk
                ps, tp = PS()
                for kc in range(8):
                    S.op("pe", lambda e, kc=kc, cbk=cbk: e.matmul(ps[:, 0:T], lhsT=wv[:, kc, cbk * 128:(cbk + 1) * 128],
                                                                   rhs=hT[:, kc, tsl], start=(kc == 0), stop=(kc == 7)),
                         reads=[tw] + T_hTt, writes=[tp])
                pb, t_pb = pre[b % 3]
                ca, t_ca = cacc[b % 3]
                S.op("act", lambda e: e.activation(out=pb[:, 3:3 + T], in_=ps[:, 0:T], func=AF.Copy),
                     reads=[tp], writes=[t_pb])
                S.op("dve", lambda e, b=b: e.tensor_copy(out=chist[l][:, b, :], in_=pb[:, T:T + 3]),
                     reads=[t_pb], writes=[T_chist[l]])
                S.op("act", lambda e, b=b: e.activation(out=ca[:, 0:T], in_=pb[:, 0:T], func=AF.Identity,
                                                        bias=cw[:, b, 4:5], scale=cw[:, b, 0:1]),
                     reads=[t_pb, T_cst], writes=[t_ca])
                if pend_silu is not None:
                    pend_silu()
                if b + 1 < 32:
                    hist_in(b + 1)
                for jj in range(1, 4):
                    S.op("dve", lambda e, b=b, jj=jj: e.scalar_tensor_tensor(out=ca[:, 0:T], in0=pb[:, jj:jj + T],
                                                                             scalar=cw[:, b, jj:jj + 1], in1=ca[:, 0:T],
                                                                             op0=ALU.mult, op1=ALU.add),
                         reads=[t_pb, t_ca, T_cst], writes=[t_ca])
                if b < 16:
                    dst, t_dst = xT[:, b, 0:T], T_xT
                elif b < 24:
                    dst, t_dst = BT[:, b - 16, 0:T], t_BT
                else:
                    dst, t_dst = CT[:, b - 24, 0:T], t_CT

                def pend_silu(dst=dst, t_dst=t_dst, ca=ca, t_ca=t_ca):
                    S.op("act", lambda e: e.activation(out=dst, in_=ca[:, 0:T], func=AF.Silu),
                         reads=[t_ca], writes=[t_dst])
                for _ in range(dt_steps.get(b, 0)):
                    next(dtg, None)
        pend_silu()
        for _ in dtg:
            pass

        arow, t_arow = A([2, 2048], BF16)
        tok4, t_tok = A([64, 4, 32])
        nhi, t_n = A([64, 32], BF16)
        nlo, _ = A([64, 32], BF16, tok=t_n)
        eab = [A([64, 32]) for _ in range(2)]
        cdb = [A([128, 32]) for _ in range(2)]
        xs, t_xs = A([64, 32, 64], BF16)
        xsd, t_xsd = A([64, 32, 64], BF16)
        Btok, t_Btok = A([64, 8, 128], BF16)
        Ebb = [A([64, 16, 64], BF16) for _ in range(2)]
        MTb = [A([64, 16, 64], BF16) for _ in range(2)]
        t1, t_t1 = A([64, 16, 64])
        ybf, t_ybf = A([64, 16, 64], BF16)
        cbs, t_cbs = A([64, 8, 64])

        def stage_P(c):
            cs = slice(c * CH, (c + 1) * CH)
            ea, t_ea = eab[c % 2]
            cd, t_cd = cdb[c % 2]
            S.dma("sp", arow[0:1, :], hiT[:, cs], D_row, reads=[t_hl], writes=[t_arow])
            S.dma("sp", arow[1:2, :], loT[:, cs], D_row, reads=[t_hl], writes=[t_arow])
            ps, tp = PS()
            for q, src in enumerate((dtT, acsT, sdtT, dAT)):
                S.op("pe", lambda e, q=q, src=src, ps=ps: e.transpose(out=ps[0:64, q * 32:(q + 1) * 32], in_=src[:, cs],
                                                                      identity=ident32[0:32, 0:32]),
                     reads=[t_dt, T_cst], writes=[tp])
            S.op("act", lambda e, ps=ps: e.activation(out=tok4[:].rearrange("p a b -> p (a b)"), in_=ps[0:64, 0:128], func=AF.Copy),
                 reads=[tp], writes=[t_tok])
            S.op("act", lambda e: e.activation(out=ea[:], in_=tok4[:, 1, :], func=AF.Exp), reads=[t_tok], writes=[t_ea])
            S.op("dve", lambda e: e.tensor_scalar(out=nhi[:], in0=tok4[:, 1, :], scalar1=-1.0, scalar2=None, op0=ALU.mult),
                 reads=[t_tok], writes=[t_n])
            S.op("dve", lambda e: e.scalar_tensor_tensor(out=nlo[:], in0=tok4[:, 1, :], scalar=-1.0, in1=nhi[:],
                                                         op0=ALU.mult, op1=ALU.subtract), reads=[t_tok], writes=[t_n])
            ps2, tp2 = PS()
            S.op("pe", lambda e: e.matmul(ps2[:, 0:32], lhsT=ones32[0:64, :], rhs=tok4[:, 3, :], start=True, stop=True),
                 reads=[t_tok, T_cst], writes=[tp2])
            S.op("act", lambda e: e.activation(out=cd[:], in_=ps2[:, 0:32], func=AF.Exp), reads=[tp2], writes=[t_cd])

        def stage_X(c):
            cs = slice(c * CH, (c + 1) * CH)
            for hh in range(2):
                ps, tp = PS()
                psb = ps[:, :].bitcast(BF16)
                for bb in range(8):
                    b = hh * 8 + bb
                    S.op("pe", lambda e, b=b, bb=bb, psb=psb: e.transpose(out=psb[0:64, bb * 128:(bb + 1) * 128],
                                                                          in_=xT[:, b, cs], identity=identb[:, :]),
                         reads=[T_xT, T_cst], writes=[tp])
                pv = psb[0:64, :].rearrange("p (h d) -> p h d", h=16)
                hs = slice(hh * 16, hh * 16 + 16)
                S.op("dve", lambda e, pv=pv, hs=hs: e.tensor_tensor(out=xs[:, hs, :], in0=pv,
                                                                    in1=bc(tok4[:, 0, hs], 2, [64, 16, 64]),
                                                                    op=ALU.mult), reads=[tp, t_tok], writes=[t_xs])
                S.op("dve", lambda e, pv=pv, hs=hs: e.tensor_tensor(out=xsd[:, hs, :], in0=pv,
                                                                    in1=bc(tok4[:, 2, hs], 2, [64, 16, 64]),
                                                                    op=ALU.mult), reads=[tp, t_tok], writes=[t_xsd])
            ps, tp = PS()
            psb = ps[:, :].bitcast(BF16)
            for g in range(8):
                S.op("pe", lambda e, g=g, psb=psb: e.transpose(out=psb[0:64, g * 128:(g + 1) * 128], in_=BT[:, g, cs],
                                                               identity=identb[:, :]), reads=[t_BT, T_cst], writes=[tp])
            S.op("act", lambda e, psb=psb: e.activation(out=Btok[:].rearrange("p g n -> p (g n)"), in_=psb[0:64, :],
                                                        func=AF.Copy), reads=[tp], writes=[t_Btok])
            psCB, tCB = PS()
            for g in range(8):
                S.op("pe", lambda e, g=g, psCB=psCB: e.matmul(psCB[0:64, g * 64:(g + 1) * 64], lhsT=BT[:, g, cs], rhs=CT[:, g, cs],
                                                              start=True, stop=True), reads=[t_BT, t_CT], writes=[tCB])
            S.op("act", lambda e, psCB=psCB: e.activation(out=cbs[:].rearrange("p g l -> p (g l)"), in_=psCB[0:64, :], func=AF.Copy),
                 reads=[tCB], writes=[t_cbs])

        def stage_S(c):
            for hh in range(2):
                Eb, t_E = Ebb[hh]
                MT, t_MT = MTb[hh]
                for bk in range(2):
                    ps, tp = PS()
                    h0 = hh * 16 + bk * 8
                    outv = ps[0:64, :].rearrange("p (h l) -> p h l", h=8)
                    mm = [
                        (onesb[0:2, 0:64], arow[0:2, h0 * 64:(h0 + 8) * 64].rearrange("p (h l) -> p h l", h=8), [t_arow]),
                        (identb[0:64, 0:64], bc(nhi[:, h0:h0 + 8], 2, [64, 8, 64]), [t_n]),
                        (identb[0:64, 0:64], bc(nlo[:, h0:h0 + 8], 2, [64, 8, 64]), [t_n]),
                        (identb[0:64, 0:64], bc(negmb[:, :], 1, [64, 8, 64]), []),
                    ]
                    for mi, (lt, rh, rd) in enumerate(mm):
                        S.op("pe", lambda e, lt=lt, rh=rh, mi=mi, outv=outv: e.matmul(outv, lhsT=lt, rhs=rh,
                                                                                       start=(mi == 0), stop=(mi == 3)),
                             reads=[T_cst] + rd, writes=[tp])
                    S.op("act", lambda e, ps=ps, bk=bk, Eb=Eb: e.activation(
                        out=Eb[:, bk * 8:(bk + 1) * 8, :].rearrange("p h l -> p (h l)"), in_=ps[0:64, :], func=AF.Exp),
                         reads=[tp], writes=[t_E])
                g0 = hh * 4
                S.op("dve", lambda e, g0=g0, MT=MT, Eb=Eb: e.tensor_tensor(
                    out=MT[:].rearrange("p (g r) l -> p g r l", g=4),
                    in0=Eb[:].rearrange("p (g r) l -> p g r l", g=4),
                    in1=bc(cbs[:, g0:g0 + 4, :], 2, [64, 4, 4, 64]), op=ALU.mult),
                     reads=[t_E, t_cbs], writes=[t_MT])

        def stage_Y(c):
            cs = slice(c * CH, (c + 1) * CH)
            ea, t_ea = eab[c % 2]
            psO, psY = {}, {}
            for hh in range(2):
                psO[hh] = [PS(), PS()]
                for gl in range(4):
                    g = hh * 4 + gl
                    pO, tO = psO[hh][gl // 2]
                    S.op("pe", lambda e, pO=pO, gl=gl, g=g: e.matmul(pO[0:64, (gl % 2) * 256:(gl % 2 + 1) * 256],
                                                                     lhsT=CT[:, g, cs], rhs=stb[l][:, g * 256:(g + 1) * 256],
                                                                     start=True, stop=True),
                         reads=[t_CT, T_stb[l]], writes=[tO])
            for hh in range(2):
                MT, t_MT = MTb[hh]
                psY[hh] = [PS(), PS()]
                for hl in range(16):
                    h = hh * 16 + hl
                    pY, tY = psY[hh][hl // 8]
                    S.op("pe", lambda e, pY=pY, hl=hl, h=h, MT=MT: e.matmul(pY[0:64, (hl % 8) * 64:(hl % 8 + 1) * 64],
                                                                            lhsT=MT[:, hl, :], rhs=xs[:, h, :], start=True, stop=True),
                         reads=[t_MT, t_xs], writes=[tY])
            for hh in range(2):
                for bk in range(2):
                    h0 = hh * 16 + bk * 8
                    pO, tO = psO[hh][bk]
                    pY, tY = psY[hh][bk]
                    S.op("dve", lambda e, pO=pO, bk=bk, h0=h0: e.tensor_tensor(
                        out=t1[:, bk * 8:(bk + 1) * 8, :], in0=pO[0:64, :].rearrange("p (h d) -> p h d", h=8),
                        in1=bc(ea[:, h0:h0 + 8], 2, [64, 8, 64]), op=ALU.mult),
                         reads=[tO, t_ea], writes=[t_t1])
                    S.op("dve", lambda e, pY=pY, bk=bk: e.tensor_tensor(
                        out=ybf[:, bk * 8:(bk + 1) * 8, :], in0=pY[0:64, :].rearrange("p (h d) -> p h d", h=8),
                        in1=t1[:, bk * 8:(bk + 1) * 8, :], op=ALU.add), reads=[tY, t_t1], writes=[t_ybf])
                ps, tp = PS()
                psb = ps[:, :].bitcast(BF16)
                for bb in range(8):
                    S.op("pe", lambda e, bb=bb, psb=psb: e.transpose(out=psb[:, bb * 64:(bb + 1) * 64],
                                                                     in_=ybf[:, 2 * bb:2 * bb + 2, :].rearrange("p h d -> p (h d)"),
                                                                     identity=identb[0:64, 0:64]), reads=[t_ybf, T_cst], writes=[tp])
                S.op("act", lambda e, psb=psb, hh=hh: e.activation(out=yT[:, hh * 8:hh * 8 + 8, cs],
                                                                   in_=psb[:, 0:512].rearrange("p (b l) -> p b l", b=8),
                                                                   func=AF.Copy), reads=[tp], writes=[T_yT])

        def stage_U(c):
            cd, t_cd = cdb[c % 2]
            psS = {}
            for hh in range(2):
                psS[hh] = [PS(), PS()]
                for gl in range(4):
                    g = hh * 4 + gl
                    pS, tS = psS[hh][gl // 2]
                    S.op("pe", lambda e, pS=pS, gl=gl, g=g: e.matmul(pS[:, (gl % 2) * 256:(gl % 2 + 1) * 256],
                                                                     lhsT=Btok[:, g, :],
                                                                     rhs=xsd[:, 4 * g:4 * g + 4, :].rearrange("p h d -> p (h d)"),
                                                                     start=True, stop=True),
                         reads=[t_Btok, t_xsd], writes=[tS])
            for hh in range(2):
                for bk in range(2):
                    h0 = hh * 16 + bk * 8
                    pS, tS = psS[hh][bk]
                    sv = st32[l][:, h0 * 64:(h0 + 8) * 64].rearrange("p (h d) -> p h d", h=8)
                    S.op("dve", lambda e, sv=sv, h0=h0: e.tensor_tensor(out=sv, in0=sv,
                                                                        in1=bc(cd[:, h0:h0 + 8], 2, [128, 8, 64]),
                                                                        op=ALU.mult), reads=[t_cd, T_st32[l]], writes=[T_st32[l]])
                    S.op("dve", lambda e, sv=sv, pS=pS: e.tensor_tensor(out=sv, in0=sv,
                                                                        in1=pS[:, :].rearrange("p (h d) -> p h d", h=8),
                                                                        op=ALU.add), reads=[tS, T_st32[l]], writes=[T_st32[l]])
            S.op("act", lambda e: e.activation(out=stb[l][:], in_=st32[l][:], func=AF.Copy),
                 reads=[T_st32[l]], writes=[T_stb[l]])

        stage_P(0)
        for c in range(nch):
            stage_X(c)
            stage_S(c)
            if c + 1 < nch:
                stage_P(c + 1)
            stage_Y(c)
            stage_U(c)

        if upto == "p1" and dbg:
            dump(tok4[:].rearrange("p a b -> p (a b)"), t_tok, 64, 128)
            dump(st32[l][:, 0:512], T_st32[l], 128, 512)
            dump16(yT[:, :, 0:64], T_yT, 128, 1024)
            dump16(xT[:, :, 0:64], T_xT, 128, 1024)
            raise _Stop()
        A_reset()
        szb = [A([128, TT]) for _ in range(4)]
        ydb = [A([128, TT]) for _ in range(4)]
        sqb = [A([128, TT], BF16) for _ in range(4)]
        vnw = vecs[:, l, 0:16]
        vdsk = vecs[:, l, 16:32]
        vpsc = vecs[:, l, 32:40]
        vbg = vecs[:, l, 40:56]
        ms_all, t_ms = A([128, 8, TT])
        pend_post = [None]

        def z_post(b, sq, t_sq, psn, tn):
            S.op("act", lambda e: e.activation(out=sq[:, 0:T], in_=yT[:, b, 0:T], func=AF.Square),
                 reads=[T_yT], writes=[t_sq])
            S.op("pe", lambda e: e.matmul(psn[:, 0:T], lhsT=onesb[:, :], rhs=sq[:, 0:T],
                                          start=(b % 2 == 0), stop=(b % 2 == 1)),
                 reads=[t_sq, T_cst], writes=[tn])
            if b % 2 == 1:
                S.op("dve", lambda e: e.tensor_scalar(out=ms_all[:, b // 2, 0:T], in0=psn[:, 0:T], scalar1=1.0 / 256.0,
                                                      scalar2=RMS_EPS, op0=ALU.mult, op1=ALU.add),
                     reads=[tn], writes=[t_ms])

        psn_cur = None
        for pi in range(4):
            wv, tw = w_in_piece(l, COL_Z + pi * 512, 512, idx=8 + pi)
            for cbk in range(4):
                b = pi * 4 + cbk
                if b % 2 == 0:
                    psn_cur = PS()
                ps, tp = PS()
                for kc in range(8):
                    S.op("pe", lambda e, kc=kc, cbk=cbk, ps=ps: e.matmul(ps[:, 0:T], lhsT=wv[:, kc, cbk * 128:(cbk + 1) * 128],
                                                                         rhs=hT[:, kc, tsl], start=(kc == 0), stop=(kc == 7)),
                         reads=[tw] + T_hTt, writes=[tp])
                sz, t_sz = szb[b % 4]
                yd, t_yd = ydb[b % 4]
                sq, t_sq = sqb[b % 4]
                S.op("dve", lambda e, b=b, yd=yd: e.scalar_tensor_tensor(out=yd[:, 0:T], in0=xT[:, b, 0:T],
                                                                         scalar=vdsk[:, b:b + 1], in1=yT[:, b, 0:T],
                                                                         op0=ALU.mult, op1=ALU.add),
                     reads=[T_xT, T_yT, T_cst], writes=[t_yd])
                S.op("act", lambda e, ps=ps, sz=sz: e.activation(out=sz[:, 0:T], in_=ps[:, 0:T], func=AF.Silu),
                     reads=[tp], writes=[t_sz])
                if pend_post[0] is not None:
                    pend_post[0]()
                S.op("dve", lambda e, b=b, yd=yd, sz=sz: e.tensor_tensor(out=yT[:, b, 0:T], in0=yd[:, 0:T], in1=sz[:, 0:T],
                                                                         op=ALU.mult), reads=[t_yd, t_sz], writes=[T_yT])
                pend_post[0] = (lambda b=b, sq=sq, t_sq=t_sq, pc=psn_cur: z_post(b, sq, t_sq, pc[0], pc[1]))
        pend_post[0]()
        msf = ms_all[:, :, :].rearrange("p g t -> p (g t)")
        S.op("act", lambda e: e.activation(out=msf, in_=msf, func=AF.Ln), reads=[t_ms], writes=[t_ms])
        S.op("act", lambda e: e.activation(out=msf, in_=msf, func=AF.Exp, scale=-0.5), reads=[t_ms], writes=[t_ms])
        for b in range(16):
            S.op("dve", lambda e, b=b: e.scalar_tensor_tensor(out=yT[:, b, 0:T], in0=yT[:, b, 0:T],
                                                              scalar=vnw[:, b:b + 1], in1=ms_all[:, b // 2, 0:T],
                                                              op0=ALU.mult, op1=ALU.mult),
                 reads=[T_yT, t_ms, T_cst], writes=[T_yT])
        if upto == "p2a":
            dump16(yT[:, :, 0:64], T_yT, 128, 1024)
            raise _Stop()
        mixp, t_mixp = A([128, 8, TT], BF16)
        ppb = [A([128, 15 + TT]) for _ in range(2)]
        pwa = [A([128, 15 + TT]) for _ in range(2)]
        pwb = [A([128, 15 + TT]) for _ in range(2)]
        for pi in range(2):
            wv, tw = w_in_piece(l, COL_POOL + pi * 512, 512, idx=12 + pi)
            for cbk in range(4):
                b = pi * 4 + cbk
                g = b // 2
                ps, tp = PS()
                for kc in range(8):
                    S.op("pe", lambda e, kc=kc, cbk=cbk, ps=ps: e.matmul(ps[:, 0:T], lhsT=wv[:, kc, cbk * 128:(cbk + 1) * 128],
                                                                         rhs=hT[:, kc, tsl], start=(kc == 0), stop=(kc == 7)),
                         reads=[tw] + T_hTt, writes=[tp])
                pp, t_pp = ppb[b % 2]
                wa, t_wa = pwa[b % 2]
                wb, t_wb = pwb[b % 2]
                S.op("dve", lambda e, b=b, pp=pp: e.tensor_copy(out=pp[:, 0:15], in_=phist[l][:, b, :]),
                     reads=[T_phist[l]], writes=[t_pp])
                S.op("act", lambda e, pp=pp, ps=ps: e.activation(out=pp[:, 15:15 + T], in_=ps[:, 0:T], func=AF.Copy),
                     reads=[tp], writes=[t_pp])
                S.op("dve", lambda e, b=b, pp=pp: e.tensor_copy(out=phist[l][:, b, :], in_=pp[:, T:T + 15]),
                     reads=[t_pp], writes=[T_phist[l]])
                src, t_src = pp, t_pp
                bufs = [(wa, t_wa), (wb, t_wb)]
                for si in range(g + 1):
                    sh = 1 << si
                    dstb, t_dstb = bufs[si % 2]
                    n = 15 + T - sh
                    S.op("dve", lambda e, src=src, dstb=dstb, sh=sh, n=n: e.tensor_tensor(
                        out=dstb[:, sh:sh + n], in0=src[:, sh:sh + n], in1=src[:, 0:n], op=ALU.add),
                         reads=[t_src], writes=[t_dstb])
                    src, t_src = dstb, t_dstb
                if ti == 0:
                    S.op("dve", lambda e, src=src, g=g: e.tensor_tensor(out=src[:, 15:15 + T], in0=src[:, 15:15 + T],
                                                                        in1=rc0[:, g, 0:T], op=ALU.mult),
                         reads=[t_src, T_cst], writes=[t_src])
                    S.op("dve", lambda e, src=src, b=b, pp=pp: e.tensor_tensor(out=mixp[:, b, 0:T], in0=src[:, 15:15 + T],
                                                                               in1=pp[:, 15:15 + T], op=ALU.subtract),
                         reads=[t_src, t_pp], writes=[t_mixp])
                else:
                    S.op("dve", lambda e, src=src, b=b, pp=pp, g=g: e.scalar_tensor_tensor(
                        out=mixp[:, b, 0:T], in0=src[:, 15:15 + T], scalar=1.0 / POOLW[g], in1=pp[:, 15:15 + T],
                        op0=ALU.mult, op1=ALU.subtract), reads=[t_src, t_pp], writes=[t_mixp])
        if upto == "p2b":
            raise _Stop()
        mixT, t_mixT = A([128, 8, TT], BF16)
        g0b = [A([128, TT]) for _ in range(2)]
        g1b = [A([128, TT]) for _ in range(2)]
        wplv = wpool_sb[:, l, :].rearrange("p (g c d) -> p g c d", g=4, c=2)
        twpl = T_cst
        w_in_v = w_in[l].rearrange("(kc p) c -> p kc c", p=128)
        w_so_v = w_so[l].rearrange("(i p) d -> p i d", p=128)
        for db in range(8):
            c0 = COL_GATE + db * 128
            pc, tws = wpiece(l, 14 + db, [
                (lambda r: r[:, 0:1024].rearrange("p (kc c) -> p kc c", kc=8), w_in_v[:, :, c0:c0 + 128]),
                (lambda r: r[:, 1024:2048].rearrange("p (kc c) -> p kc c", kc=8), w_in_v[:, :, c0 + 1024:c0 + 1024 + 128]),
                (lambda r: r[:, 2048:4096].rearrange("p (i d) -> p i d", i=16), w_so_v[:, :, db * 128:(db + 1) * 128]),
            ])
            wg0 = pc[:, 0:1024].rearrange("p (kc c) -> p kc c", kc=8)
            wg1 = pc[:, 1024:2048].rearrange("p (kc c) -> p kc c", kc=8)
            wsv = pc[:, 2048:4096].rearrange("p (i d) -> p i d", i=16)
            g0, t_g0 = g0b[db % 2]
            g1, t_g1 = g1b[db % 2]
            for (wv, gg, t_gg, bcol) in ((wg0, g0, t_g0, db), (wg1, g1, t_g1, 8 + db)):
                ps, tp = PS()
                for kc in range(8):
                    S.op("pe", lambda e, kc=kc, ps=ps, wv=wv: e.matmul(ps[:, 0:T], lhsT=wv[:, kc, :],
                                                                       rhs=hT[:, kc, tsl], start=(kc == 0), stop=(kc == 7)),
                         reads=[tws] + T_hTt, writes=[tp])
                S.op("act", lambda e, ps=ps, gg=gg, bcol=bcol: e.activation(out=gg[:, 0:T], in_=ps[:, 0:T], func=AF.Sigmoid,
                                                                            bias=vbg[:, bcol:bcol + 1], scale=1.0),
                     reads=[tp, T_cst], writes=[t_gg])
            psy, tpy = PS()
            for i in range(16):
                S.op("pe", lambda e, i=i, psy=psy, wsv=wsv: e.matmul(psy[:, 0:T], lhsT=wsv[:, i, :],
                                                                     rhs=yT[:, i, 0:T], start=(i == 0), stop=(i == 15)),
                     reads=[tws, T_yT], writes=[tpy])
            psp, tpp = PS()
            gp = db // 2
            for c2 in range(2):
                S.op("pe", lambda e, c2=c2, psp=psp, gp=gp, db=db: e.matmul(psp[:, 0:T],
                                                                            lhsT=wplv[:, gp, c2, (db % 2) * 128:(db % 2 + 1) * 128],
                                                                            rhs=mixp[:, gp * 2 + c2, 0:T], start=(c2 == 0), stop=(c2 == 1)),
                     reads=[twpl, t_mixp], writes=[tpp])
            S.op("dve", lambda e, g0=g0, psy=psy: e.tensor_tensor(out=g0[:, 0:T], in0=g0[:, 0:T], in1=psy[:, 0:T], op=ALU.mult),
                 reads=[tpy, t_g0], writes=[t_g0])
            S.op("dve", lambda e, g1=g1, psp=psp, db=db: e.scalar_tensor_tensor(out=g1[:, 0:T], in0=psp[:, 0:T],
                                                                                scalar=vpsc[:, db:db + 1], in1=g1[:, 0:T],
                                                                                op0=ALU.mult, op1=ALU.mult),
                 reads=[tpp, t_g1, T_cst], writes=[t_g1])
            S.op("dve", lambda e, g0=g0, g1=g1, db=db: e.tensor_tensor(out=mixT[:, db, 0:T], in0=g0[:, 0:T], in1=g1[:, 0:T], op=ALU.add),
                 reads=[t_g0, t_g1], writes=[t_mixT])
        if upto == "p2c":
            dump16(mixT[:, :, 0:64], t_mixT, 128, 512)
            dump16(mixp[:, :, 0:64], t_mixp, 128, 512)
            for q_ in range(2):
                dump(g0b[q_][0][:, 0:64], g0b[q_][1], 128, 64)
                dump(g1b[q_][0][:, 0:64], g1b[q_][1], 128, 64)
            raise _Stop()
        wo = [wpiece(l, 22 + hf, [(lambda r: r[:, :].rearrange("p (k d) -> p k d", k=8),
                                   w_out[l].rearrange("(k p) d -> p k d", p=128)[:, :, hf * 512:(hf + 1) * 512])]) for hf in range(2)]
        if first_tile:
            load_lnt(0, 2 + 4 * l)
            load_lnt(1, 3 + 4 * l)
        for (j, rows, btok) in subs:
            tl = btok - btok0
            for hf in range(2):
                pc, tk = wo[hf]
                wov = pc[:, :].rearrange("p (k d) -> p k d", k=8)
                ps, tp = PS()
                for k in range(8):
                    S.op("pe", lambda e, k=k, ps=ps, wov=wov: e.matmul(ps[0:rows, :], lhsT=mixT[:, k, tl:tl + rows], rhs=wov[:, k, :],
                                                                       start=(k == 0), stop=(k == 7)),
                         reads=[tk, t_mixT], writes=[tp])
                hv = hres[0:rows, j, hf * 512:(hf + 1) * 512]
                S.op("dve", lambda e, hv=hv, ps=ps: e.scalar_tensor_tensor(out=hv, in0=hv, scalar=ALPHA, in1=ps[0:rows, :],
                                                                           op0=ALU.mult, op1=ALU.add),
                     reads=[tp, T_hres[j]], writes=[T_hres[j]])
        for (j, rows, btok) in subs:
            if upto == "p2e":
                continue
            layer_norm_inplace(j, rows)
        for (j, rows, btok) in subs:
            if upto in ("p2e", "p2f"):
                continue
            transpose_to_hT(j, rows, btok, router=(upto != "p2d"), mask0=False)

    def moe(l, allsubs, ntok):
        A_reset()
        load_lnt(0, 4 + 4 * l)
        load_lnt(1, 5 + 4 * l)
        mt = []
        cur = []
        for s_ in allsubs:
            if cur and (s_[2] + s_[1] - cur[0][2]) > 512:
                mt.append(cur)
                cur = []
            cur.append(s_)
        if cur:
            mt.append(cur)
        hm, t_hm = A([128, 4, 512], BF16)
        sgb = [A([128, 512]) for _ in range(2)]
        for (j, rows, btok) in allsubs:
            S.op("act", lambda e, j=j, rows=rows: e.activation(out=hres[0:rows, j, :], in_=hres[0:rows, j, :], func=AF.Copy,
                                                               scale=ALPHA), reads=[T_hres[j]], writes=[T_hres[j]])
        for ex in range(NE):
            wg, twg = wload([(lambda r: r[:, :].rearrange("p (k f) -> p k f", k=8),
                              w_eg[l, ex].rearrange("(k p) f -> p k f", p=128))])
            wu, twu = wload([(lambda r: r[:, :].rearrange("p (k f) -> p k f", k=8),
                              w_eu[l, ex].rearrange("(k p) f -> p k f", p=128))])
            wd, twd = wload([(lambda r: r[:, :].rearrange("p (k d) -> p k d", k=4),
                              w_ed[l, ex].rearrange("(k p) d -> p k d", p=128))])
            wgv = wg[:, :].rearrange("p (k f) -> p k f", k=8)
            wuv = wu[:, :].rearrange("p (k f) -> p k f", k=8)
            wdv = wd[:, :].rearrange("p (k d) -> p k d", k=4)
            for grp in mt:
                b0 = grp[0][2]
                n = grp[-1][2] + grp[-1][1] - b0
                rd_hT = [T_hT[j] for j, _, _ in grp]
                for fb in range(4):
                    psg, tg = PS()
                    psu, tu = PS()
                    for kc in range(8):
                        S.op("pe", lambda e, kc=kc, fb=fb, psg=psg: e.matmul(psg[:, 0:n], lhsT=wgv[:, kc, fb * 128:(fb + 1) * 128],
                                                                             rhs=hT[:, kc, b0:b0 + n], start=(kc == 0), stop=(kc == 7)),
                             reads=[twg] + rd_hT, writes=[tg])
                    for kc in range(8):
                        S.op("pe", lambda e, kc=kc, fb=fb, psu=psu: e.matmul(psu[:, 0:n], lhsT=wuv[:, kc, fb * 128:(fb + 1) * 128],
                                                                             rhs=hT[:, kc, b0:b0 + n], start=(kc == 0), stop=(kc == 7)),
                             reads=[twu] + rd_hT, writes=[tu])
                    sg, t_sg = sgb[fb % 2]
                    S.op("act", lambda e, psg=psg, sg=sg: e.activation(out=sg[:, 0:n], in_=psg[:, 0:n], func=AF.Silu),
                         reads=[tg], writes=[t_sg])
                    S.op("dve", lambda e, psu=psu, sg=sg, fb=fb: e.tensor_tensor(out=hm[:, fb, 0:n], in0=sg[:, 0:n], in1=psu[:, 0:n],
                                                                                 op=ALU.mult), reads=[tu, t_sg], writes=[t_hm])
                for (j, rows, btok) in grp:
                    tl = btok - b0
                    for hf in range(2):
                        ps, tp = PS()
                        for fb in range(4):
                            S.op("pe", lambda e, fb=fb, ps=ps, tl=tl, rows=rows, hf=hf: e.matmul(
                                ps[0:rows, :], lhsT=hm[:, fb, tl:tl + rows], rhs=wdv[:, fb, hf * 512:(hf + 1) * 512],
                                start=(fb == 0), stop=(fb == 3)), reads=[t_hm, twd], writes=[tp])
                        hv = hres[0:rows, j, hf * 512:(hf + 1) * 512]
                        S.op("dve", lambda e, hv=hv, ps=ps, j=j, rows=rows, ex=ex: e.scalar_tensor_tensor(
                            out=hv, in0=ps[0:rows, :], scalar=comb[0:rows, j, ex:ex + 1], in1=hv, op0=ALU.mult, op1=ALU.add),
                             reads=[tp, T_hres[j], T_comb[j]], writes=[T_hres[j]])
        for (j, rows, btok) in allsubs:
            layer_norm_inplace(j, rows)
            if l + 1 < nlayers:
                transpose_to_hT(j, rows, btok, router=False, mask0=(rows == 64))

    for bi in range(nblocks):
        A_reset()
        tl_list = blocks[bi]
        allsubs = []
        tile_subs = []
        btok = 0
        j = 0
        for ti in tl_list:
            slot0, T = tiles[ti]
            subs = []
            nsub = max(1, T // 128)
            for s_ in range(nsub):
                rows = min(128, T)
                subs.append((j, rows, btok))
                S.dma("sp", hres[0:rows, j, :], xin[slot0 + s_ * 128: slot0 + s_ * 128 + rows, :], D_x[j], writes=[T_hres[j]])
                btok += rows
                j += 1
            tile_subs.append((ti, subs, subs[0][2]))
            allsubs += subs
        try:
            load_lnt(0, 0)
            load_lnt(1, 1)
            for (j, rows, bt) in allsubs:
                layer_norm_inplace(j, rows)
                transpose_to_hT(j, rows, bt, router=False, mask0=(rows == 64))
            if upto == "ln0":
                raise _Stop()
            for l in range(nlayers):
                for k, (ti, subs, bt0) in enumerate(tile_subs):
                    mixer(l, ti, subs, bt0, first_tile=(k == 0))
                    wsc_ready[l] = True
                    if upto in ("tile0", "p2d", "p2e", "p2f"):
                        raise _Stop()
                if upto == "mixer":
                    raise _Stop()
                moe(l, allsubs, btok)
        except _Stop:
            for (j, rows, bt) in allsubs:
                S.dma("sp", out_d[bt:bt + rows, :], hres[0:rows, j, :], D_out, reads=[T_hres[j]], writes=[T_out])
            break
        for (j, rows, bt) in allsubs:
            if rows == 64:
                continue
            orow = tiles[tl_list[0]][0] + bt - 64
            S.dma("sp", out_d[orow:orow + rows, :], hres[0:rows, j, :], D_out, reads=[T_hres[j]], writes=[T_out])
    S.wait_all("sp", [D_out, D_dbg] + D_ring)
    return nc


def _consts():
    c = np.zeros((128, 1024), np.float32)
    c[:, 0:128] = np.eye(128, dtype=np.float32)
    c[:, 128:256] = 1.0
    t = np.arange(64)
    c[0:64, 256:320] = (t[:, None] <= t[None, :]).astype(np.float32)
    c[0:64, 320:384] = np.where(t[None, :] < t[:, None], -30000.0, 0.0).astype(np.float32)
    rc = np.zeros((4, 64), np.float32)
    for g, w in enumerate(POOLW):
        for s in range(64):
            tt = s - 48
            rc[g, s] = 1.0 / w if tt < 0 else 1.0 / min(tt + 1, w)
    c[:, 384:640] = rc.reshape(1, 256)
    c[0:64, 640] = (t >= 48).astype(np.float32)
    c[:, 641] = LN_EPS
    c[:, 642] = RMS_EPS
    c[:, 643] = 1.0
    return c


_NC_CACHE = {}


def kernel(x, meta_tokens, ln_in_g, ln_in_b, w_router, w_in, conv_w, conv_b, dt_bias, a_log, d_skip, ssd_norm_w,
           w_ssd_out, w_pool, pool_scale, b_gate, w_out, ln1_g, ln1_b, w_exp_gate, w_exp_up, w_exp_down, ln2_g, ln2_b,
           _cfg=None):
    f = lambda a: np.ascontiguousarray(np.asarray(a, dtype=np.float32))
    x = f(x)
    cfg = _cfg or {}
    ncores = cfg.get("ncores", 8)
    key = tuple(sorted(cfg.items()))
    if key not in _NC_CACHE:
        _NC_CACHE[key] = build(**cfg)
    nc = _NC_CACHE[key]
    lnrows = [ln_in_g, ln_in_b]
    for l in range(2):
        lnrows += [ln1_g[l], ln1_b[l], ln2_g[l], ln2_b[l]]
    lnt = np.ascontiguousarray(np.broadcast_to(np.stack([f(r) for r in lnrows])[:, None, :], (10, 128, D)))
    convw = np.zeros((2, 128, 32, 5), np.float32)
    vecs = np.zeros((2, 128, 64), np.float32)
    cw, cb = f(conv_w), f(conv_b)
    for l in range(2):
        convw[l, :, :, 0:4] = cw[l].reshape(4, 32, 128).transpose(2, 1, 0)
        convw[l, :, :, 4] = cb[l].reshape(32, 128).T
        vecs[l, :, 0:16] = f(ssd_norm_w)[l].reshape(16, 128).T
        vecs[l, :, 16:32] = np.repeat(f(d_skip)[l], 64).reshape(16, 128).T
        vecs[l, :, 32:40] = f(pool_scale)[l].reshape(8, 128).T
        vecs[l, :, 40:56] = f(b_gate)[l].reshape(16, 128).T
        vecs[l, 0:32, 56] = f(dt_bias)[l]
        vecs[l, 0:32, 57] = f(a_log)[l]
    wr = np.ascontiguousarray(f(w_router).reshape(8, 128, 16).transpose(1, 0, 2))
    shared = {
        "w_in": f(w_in), "w_so": f(w_ssd_out), "w_pool": f(w_pool), "w_out": f(w_out),
        "w_eg": f(w_exp_gate), "w_eu": f(w_exp_up), "w_ed": f(w_exp_down),
        "lnt": lnt, "cst": _consts(), "convw": convw, "vecs": vecs, "wr": wr,
    }
    in_maps = []
    meta = f(meta_tokens)
    for c in range(ncores):
        b = c % 4
        xin = np.zeros((NSLOT, D), np.float32)
        xin[48:64] = meta
        xin[64:] = x[b]
        m = dict(shared)
        m["xin"] = xin
        in_maps.append(m)
    tr = bool(cfg.get("trace"))
    res = run_bass_kernel_spmd(nc, in_maps, core_ids=list(range(ncores)), **({"trace": True} if tr else {}))
    if tr:
        print("EXEC_TIME_NS", res.exec_time_ns)
    out = np.stack([np.asarray(res.results[b]["out"], dtype=np.float32) for b in range(min(4, ncores))], axis=0)
    if cfg.get("dbg"):
        kernel.last_dbg = [np.asarray(res.results[b]["dbg"]) for b in range(min(4, ncores))]
        kernel.last_dbgb = [np.asarray(res.results[b]["dbgb"]).astype(np.float32) for b in range(min(4, ncores))]
    return out
```

```python
import numpy as np
import concourse.bass as bass
import concourse.mybir as mybir
from concourse.bass_utils import run_bass_kernel_spmd

F32 = mybir.dt.float32
BF16 = mybir.dt.bfloat16
U8 = mybir.dt.uint8
AF = mybir.ActivationFunctionType
ALU = mybir.AluOpType
AX = mybir.AxisListType

D = 1024
NSLOT = 4160
CH = 64
TT = 256
NH = 32
NG = 8
COL_Z, COL_X, COL_B, COL_C, COL_DT, COL_POOL, COL_GATE, INC = 0, 2048, 4096, 5120, 6144, 6176, 7200, 9248
NE = 16
ALPHA = 4.0 ** 0.25
LN_EPS = 1e-5
RMS_EPS = 1e-5
NO_SELF_SYNC = False
NW = 4
ARENA = 55296
POOLW = (2, 4, 8, 16)


class Tok:
    __slots__ = ("w", "r", "name")

    def __init__(self, name=""):
        self.w = None
        self.r = {}
        self.name = name


class _Eng:
    def __init__(self, name, e, sem):
        self.name, self.e, self.sem, self.cnt, self.known = name, e, sem, 0, {}


class _DSem:
    def __init__(self, h):
        self.h, self.cnt = h, 0


class Sched:
    def __init__(self, nc):
        self.nc = nc
        self.nsem = 0
        self.engs = {}
        for name, e in (("pe", nc.tensor), ("act", nc.scalar), ("dve", nc.vector), ("pool", nc.gpsimd),
                        ("sp", nc.sync)):
            self.engs[name] = _Eng(name, e, self._sem("s_" + name))

    def _sem(self, name):
        self.nsem += 1
        return self.nc.semaphore(name).__enter__()

    def dsem(self, name):
        return _DSem(self._sem("d_" + name))

    def _wait(self, E, deps):
        best = {}
        for sem, val in deps:
            k = id(sem)
            if k not in best or best[k][1] < val:
                best[k] = (sem, val)
        for k, (sem, val) in best.items():
            if sem is E.sem and (E.name == "pe" or NO_SELF_SYNC):
                continue
            if E.known.get(k, 0) >= val:
                continue
            E.e.wait_ge(sem, val)
            E.known[k] = val

    @staticmethod
    def _deps(reads, writes):
        deps = []
        for t in reads:
            if t.w is not None:
                deps.append(t.w)
        for t in writes:
            if t.w is not None:
                deps.append(t.w)
            deps.extend(t.r.values())
        return deps

    def op(self, eng, fn, reads=(), writes=()):
        E = self.engs[eng]
        self._wait(E, self._deps(reads, writes))
        ins = fn(E.e)
        E.cnt += 1
        ins.then_inc(E.sem, 1)
        mark = (E.sem, E.cnt)
        for t in writes:
            t.w = mark
            t.r = {}
        for t in reads:
            if t.w is not mark:
                t.r[id(E.sem)] = mark

    def dma(self, queue, out, in_, ds, reads=(), writes=(), **kw):
        Q = self.engs[queue]
        self._wait(Q, self._deps(reads, writes))
        ins = Q.e.dma_start(out=out, in_=in_, **kw)
        ds.cnt += 16
        ins.then_inc(ds.h, 16)
        mark = (ds.h, ds.cnt)
        for t in writes:
            t.w = mark
            t.r = {}
        for t in reads:
            t.r[id(ds.h)] = mark

    def barrier(self, names=("pe", "act", "dve", "pool")):
        for a in names:
            A = self.engs[a]
            deps = [(self.engs[b].sem, self.engs[b].cnt) for b in names if b != a and self.engs[b].cnt > 0]
            self._wait(A, deps)

    def wait_all(self, eng, dsems):
        E = self.engs[eng]
        deps = [(x.sem, x.cnt) for x in self.engs.values() if x is not E and x.cnt > 0]
        deps += [(d.h, d.cnt) for d in dsems if d.cnt > 0]
        self._wait(E, deps)


def bc(ap, axis, shape):
    return ap.unsqueeze(axis).broadcast_to(list(shape))


def seq_tiles():
    tiles = [(0, 64)] + [(64 + i * TT, TT) for i in range((NSLOT - 64) // TT)]
    blocks = [[0, 1, 2, 3, 4]] + [list(range(5 + 4 * i, 9 + 4 * i)) for i in range(3)]
    return tiles, blocks


class _Stop(Exception):
    pass


def build(nblocks=4, nlayers=2, dbg=False, upto="full", ncores=8, trace=False):
    nc = bass.Bass("TRN2", target_bir_lowering=False)
    S = Sched(nc)

    def dram(name, shape, dt=F32, kind="ExternalInput"):
        return nc.dram_tensor(name, list(shape), dt, kind=kind).ap()

    xin = dram("xin", [NSLOT, D])
    w_in = dram("w_in", [2, D, INC])
    w_so = dram("w_so", [2, 2048, D])
    w_pool = dram("w_pool", [2, 4, 256, 256])
    w_out = dram("w_out", [2, D, D])
    w_eg = dram("w_eg", [2, NE, D, 512])
    w_eu = dram("w_eu", [2, NE, D, 512])
    w_ed = dram("w_ed", [2, NE, 512, D])
    lnt_d = dram("lnt", [10, 128, D])
    cst_d = dram("cst", [128, 1024])
    convw_d = dram("convw", [2, 128, 32, 5])
    vec_d = dram("vecs", [2, 128, 64])
    wr_d = dram("wr", [128, 8, 16])
    out_d = dram("out", [4096, D], kind="ExternalOutput")
    dbg_d = dram("dbg", [128, 8192], kind="ExternalOutput") if dbg else None
    dbgb_d = dram("dbgb", [128, 8192], BF16, kind="ExternalOutput") if dbg else None

    def sb(name, shape, dt=F32):
        return nc.alloc_sbuf_tensor(name, list(shape), dt)

    hres = sb("hres", [128, 9, D])
    hT = sb("hT", [128, 8, 1088], BF16)
    xT = sb("xT", [128, 16, TT], BF16)
    yT = sb("yT", [128, 16, TT], BF16)
    st32 = [sb(f"st32_{l}", [128, 2048]) for l in range(2)]
    stb = [sb(f"stb_{l}", [128, 2048], BF16) for l in range(2)]
    chist = [sb(f"chist_{l}", [128, 32, 3]) for l in range(2)]
    phist = [sb(f"phist_{l}", [128, 8, 15]) for l in range(2)]
    lnt = sb("lnt_sb", [128, 2, D])
    ring = [sb(f"ring{i}", [128, 4096], BF16) for i in range(NW)]
    cst = sb("cst_sb", [128, 1024])
    convw = sb("convw_sb", [128, 2, 32, 5])
    vecs = sb("vecs_sb", [128, 2, 64])
    wr = sb("wr_sb", [128, 8, 16])
    comb = sb("comb", [128, 9, 16])
    identb = sb("identb", [128, 128], BF16)
    onesb = sb("onesb", [128, 128], BF16)
    negmb = sb("negmb", [64, 64], BF16)
    Aneg = sb("Aneg", [32, 2])
    wrh = sb("wrh", [128, 8, 16], BF16)
    wrl = sb("wrl", [128, 8, 16], BF16)
    wpool_sb = sb("wpool_sb", [128, 2, 2048], BF16)
    arena = sb("arena", [128, ARENA], U8)

    T_hres = [Tok(f"hres{j}") for j in range(9)]
    T_hT = [Tok(f"hT{j}") for j in range(9)]
    T_xT, T_yT = Tok("xT"), Tok("yT")
    T_st32 = [Tok(), Tok()]
    T_stb = [Tok(), Tok()]
    T_chist = [Tok(), Tok()]
    T_phist = [Tok(), Tok()]
    T_lnt = [Tok(), Tok()]
    T_ring = [Tok(f"ring{i}") for i in range(NW)]
    T_cst = Tok("cst")
    T_comb = [Tok() for _ in range(9)]
    T_out = Tok("out")

    D_ring = [S.dsem(f"ring{i}") for i in range(NW)]
    D_in = S.dsem("in")
    D_x = [S.dsem(f"x{j}") for j in range(9)]
    D_lnt = [S.dsem("lnt0"), S.dsem("lnt1")]
    D_out = S.dsem("out")
    D_row = S.dsem("arow")
    D_dbg = S.dsem("dbg")

    ident32 = cst[:, 0:128]
    ones32 = cst[:, 128:256]
    tri32 = cst[0:64, 256:320]
    negm32 = cst[0:64, 320:384]
    rc0 = cst[:, 384:640].rearrange("p (g l) -> p g l", g=4)
    m0 = cst[0:64, 640:641]
    epsc = cst[:, 641:644]

    psum = [nc.alloc_psum_tensor(f"ps{i}", [128, 512], F32) for i in range(8)]
    T_ps = [Tok(f"ps{i}") for i in range(8)]
    ps_rr = [0]

    def PS():
        i = ps_rr[0]
        ps_rr[0] = (i + 1) % 8
        return psum[i], T_ps[i]

    aoff = [0]
    a_live = []
    a_old = []

    def A_reset(barrier=False):
        if barrier:
            S.barrier()
        a_old.extend(a_live)
        del a_live[:]
        aoff[0] = 0

    def A(shape, dt=F32, tok=None):
        esz = {F32: 4, BF16: 2}[dt]
        n = 1
        for s in shape[1:]:
            n *= s
        nb = (n * esz + 31) // 32 * 32
        o = aoff[0]
        aoff[0] += nb
        assert aoff[0] <= ARENA, f"arena overflow {aoff[0]}"
        v = arena[0:shape[0], o:o + n * esz].bitcast(dt)
        if len(shape) == 3:
            v = v.rearrange("p (a b) -> p a b", a=shape[1])
        elif len(shape) == 4:
            v = v.rearrange("p (a b c) -> p a b c", a=shape[1], b=shape[2])
        tk = tok if tok is not None else Tok()
        keep = []
        for (s0, e0, t0) in a_old:
            if e0 <= o or s0 >= o + nb:
                keep.append((s0, e0, t0))
                continue
            marks = list(t0.r.values())
            if t0.w is not None:
                marks.append(t0.w)
            for (sem, val) in marks:
                k = id(sem)
                if k not in tk.r or tk.r[k][1] < val:
                    tk.r[k] = (sem, val)
            if not (s0 >= o and e0 <= o + nb):
                keep.append((s0, e0, t0))
        a_old[:] = keep
        a_live.append((o, o + nb, tk))
        return v, tk

    dbg_col = [0]

    def dump(ap, tok, rows, ncols):
        if not dbg:
            return
        c = dbg_col[0]
        dbg_col[0] += ncols
        S.dma("sp", dbg_d[0:rows, c:c + ncols], ap, D_dbg, reads=[tok])
        return c

    dbgb_col = [0]

    def dump16(ap, tok, rows, ncols):
        if not dbg:
            return
        c = dbgb_col[0]
        dbgb_col[0] += ncols
        S.dma("sp", dbgb_d[0:rows, c:c + ncols], ap, D_dbg, reads=[tok])

    ring_i = [0]

    def wload(parts):
        i = ring_i[0]
        ring_i[0] = (i + 1) % NW
        for dst_fn, src in parts:
            S.dma("pool", dst_fn(ring[i]), src, D_ring[i], writes=[T_ring[i]])
        return ring[i], T_ring[i]

    NPC = 24
    wsc = dram("wsc", [2 * NPC, 128, 4096], BF16, kind="Internal")
    T_wsc = [Tok("wsc0"), Tok("wsc1")]
    D_wst = S.dsem("wst")
    wsc_ready = [False, False]

    def wpiece(l, idx, parts):
        if not wsc_ready[l]:
            pc, tk = wload(parts)
            S.dma("sp", wsc[l * NPC + idx], pc[:, :], D_wst, reads=[tk])
            T_wsc[l].w = (D_wst.h, D_wst.cnt)
            return pc, tk
        i = ring_i[0]
        ring_i[0] = (i + 1) % NW
        S.dma("pool", ring[i][:, :], wsc[l * NPC + idx], D_ring[i], reads=[T_wsc[l]], writes=[T_ring[i]])
        return ring[i], T_ring[i]

    def w_in_piece(l, c0, ncol, idx=None):
        src = w_in[l].rearrange("(kc p) c -> p kc c", p=128)[:, :, c0:c0 + ncol]
        parts = [(lambda r: r[:, 0:8 * ncol].rearrange("p (kc c) -> p kc c", kc=8), src)]
        pc, tk = wload(parts) if idx is None else wpiece(l, idx, parts)
        return pc[:, 0:8 * ncol].rearrange("p (kc c) -> p kc c", kc=8), tk

    S.dma("sp", cst[:], cst_d[:, :], D_in, writes=[T_cst])
    S.dma("sp", convw[:], convw_d.rearrange("l p b j -> p l b j"), D_in, writes=[T_cst])
    S.dma("sp", vecs[:], vec_d.rearrange("l p c -> p l c"), D_in, writes=[T_cst])
    S.dma("sp", wr[:], wr_d[:, :, :], D_in, writes=[T_cst])
    S.op("act", lambda e: e.activation(out=identb[:], in_=ident32, func=AF.Copy), reads=[T_cst], writes=[T_cst])
    S.op("act", lambda e: e.activation(out=onesb[:], in_=ones32, func=AF.Copy), reads=[T_cst], writes=[T_cst])
    S.op("act", lambda e: e.activation(out=negmb[:], in_=negm32, func=AF.Copy), reads=[T_cst], writes=[T_cst])
    S.op("act", lambda e: e.activation(out=wrh[:], in_=wr[:], func=AF.Copy), reads=[T_cst], writes=[T_cst])
    S.op("dve", lambda e: e.tensor_tensor(out=wrl[:], in0=wr[:], in1=wrh[:], op=ALU.subtract), reads=[T_cst], writes=[T_cst])
    S.op("act", lambda e: e.activation(out=Aneg[:, 0:1], in_=vecs[0:32, 0, 57:58], func=AF.Exp),
         reads=[T_cst], writes=[T_cst])
    S.op("act", lambda e: e.activation(out=Aneg[:, 1:2], in_=vecs[0:32, 1, 57:58], func=AF.Exp),
         reads=[T_cst], writes=[T_cst])
    S.op("dve", lambda e: e.tensor_scalar(out=Aneg[:], in0=Aneg[:], scalar1=-1.0, scalar2=None, op0=ALU.mult),
         reads=[T_cst], writes=[T_cst])
    D_wp = S.dsem("wpool")
    for l in range(2):
        S.dma("pool", wpool_sb[:, l, :].rearrange("p (g c d) -> p g c d", g=4, c=2),
              w_pool[l].rearrange("g (c p) d -> p g c d", p=128), D_wp, writes=[T_cst])
    for l in range(2):
        S.op("dve", lambda e, l=l: e.memset(st32[l][:], 0.0), writes=[T_st32[l]])
        S.op("dve", lambda e, l=l: e.memset(stb[l][:], 0.0), writes=[T_stb[l]])
        S.op("dve", lambda e, l=l: e.memset(chist[l][:], 0.0), writes=[T_chist[l]])
        S.op("dve", lambda e, l=l: e.memset(phist[l][:], 0.0), writes=[T_phist[l]])

    tiles, blocks = seq_tiles()

    def load_lnt(slot, idx):
        S.dma("sp", lnt[:, slot, :], lnt_d[idx], D_lnt[slot], writes=[T_lnt[slot]])

    def layer_norm_inplace(j, rows, pre_fn=None):
        h = hres[0:rows, j, :]
        tk = T_hres[j]
        stats, t_s = A([128, 2, 6])
        mv, _ = A([128, 2], tok=t_s)
        rstd, _ = A([128, 1], tok=t_s)
        nb, _ = A([128, 1], tok=t_s)
        for c in range(2):
            S.op("dve", lambda e, c=c: e.bn_stats(out=stats[0:rows, c, :], in_=h[:, c * 512:(c + 1) * 512]),
                 reads=[tk], writes=[t_s])
        S.op("dve", lambda e: e.bn_aggr(out=mv[0:rows, :], in_=stats[0:rows, :, :].rearrange("p a b -> p (a b)")), reads=[t_s], writes=[t_s])
        S.op("act", lambda e: e.activation(out=rstd[0:rows, :], in_=mv[0:rows, 1:2], func=AF.Ln, bias=epsc[0:rows, 0:1], scale=1.0),
             reads=[t_s, T_cst], writes=[t_s])
        S.op("act", lambda e: e.activation(out=rstd[0:rows, :], in_=rstd[0:rows, :], func=AF.Exp, scale=-0.5),
             reads=[t_s], writes=[t_s])
        S.op("dve", lambda e: e.scalar_tensor_tensor(out=nb[0:rows, :], in0=mv[0:rows, 0:1], scalar=-1.0,
                                                     in1=rstd[0:rows, :], op0=ALU.mult, op1=ALU.mult),
             reads=[t_s], writes=[t_s])
        S.op("act", lambda e: e.activation(out=h, in_=h, func=AF.Identity, bias=nb[0:rows, :], scale=rstd[0:rows, :]),
             reads=[tk, t_s], writes=[tk])
        S.op("dve", lambda e: e.tensor_tensor(out=h, in0=h, in1=lnt[0:rows, 0, :], op=ALU.mult),
             reads=[tk, T_lnt[0]], writes=[tk])
        S.op("dve", lambda e: e.tensor_tensor(out=h, in0=h, in1=lnt[0:rows, 1, :], op=ALU.add),
             reads=[tk, T_lnt[1]], writes=[tk])

    def transpose_to_hT(j, rows, btok, router, mask0):
        tk = T_hres[j]
        if mask0:
            S.op("dve", lambda e: e.tensor_scalar(out=hres[0:rows, j, :], in0=hres[0:rows, j, :], scalar1=m0,
                                                  scalar2=None, op0=ALU.mult), reads=[tk, T_cst], writes=[tk])
        hb, t_hb = A([128, D], BF16)
        S.op("act", lambda e: e.activation(out=hb[0:rows, :], in_=hres[0:rows, j, :], func=AF.Copy), reads=[tk], writes=[t_hb])
        ps, tp = PS()
        psb = ps[:, :].bitcast(BF16)
        for kc in range(8):
            S.op("pe", lambda e, kc=kc, psb=psb: e.transpose(out=psb[:, kc * 128:kc * 128 + rows],
                                                             in_=hb[0:rows, kc * 128:(kc + 1) * 128],
                                                             identity=identb[0:rows, 0:rows]),
                 reads=[t_hb, T_cst], writes=[tp])
        S.op("act", lambda e, psb=psb: e.activation(out=hT[:, :, btok:btok + rows],
                                                    in_=psb[:, :].rearrange("p (a b) -> p a b", a=8)[:, :, 0:rows], func=AF.Copy),
             reads=[tp], writes=[T_hT[j]])
        if not router:
            return
        hlo, t_hlo = A([128, D], BF16)
        hTlo, t_hTlo = A([128, 8, 128], BF16)
        S.op("dve", lambda e: e.tensor_tensor(out=hlo[0:rows, :], in0=hres[0:rows, j, :], in1=hb[0:rows, :], op=ALU.subtract),
             reads=[tk, t_hb], writes=[t_hlo])
        ps, tp = PS()
        psb = ps[:, :].bitcast(BF16)
        for kc in range(8):
            S.op("pe", lambda e, kc=kc, psb=psb: e.transpose(out=psb[:, kc * 128:kc * 128 + rows],
                                                             in_=hlo[0:rows, kc * 128:(kc + 1) * 128],
                                                             identity=identb[0:rows, 0:rows]),
                 reads=[t_hlo, T_cst], writes=[tp])
        S.op("dve", lambda e, psb=psb: e.tensor_copy(out=hTlo[:, :, 0:rows],
                                                     in_=psb[:, :].rearrange("p (a b) -> p a b", a=8)[:, :, 0:rows]),
             reads=[tp], writes=[t_hTlo])
        ps, tp = PS()
        n = 0
        for kc in range(8):
            for (lt, rd, rh) in ((hT[:, kc, btok:btok + rows], T_hT[j], wrh[:, kc, :]),
                                 (hT[:, kc, btok:btok + rows], T_hT[j], wrl[:, kc, :]),
                                 (hTlo[:, kc, 0:rows], t_hTlo, wrh[:, kc, :])):
                S.op("pe", lambda e, lt=lt, rh=rh, n=n, ps=ps: e.matmul(ps[0:rows, 0:16], lhsT=lt, rhs=rh, start=(n == 0), stop=(n == 23)),
                     reads=[rd, T_cst], writes=[tp])
                n += 1
        R = slice(0, rows)
        mx, t_k = A([128, 1])
        ee, _ = A([128, 4, 4], tok=t_k)
        m1, _ = A([128, 4], tok=t_k)
        eq1, _ = A([128, 4, 4], tok=t_k)
        e2, _ = A([128, 4, 4], tok=t_k)
        m2, _ = A([128, 4], tok=t_k)
        eq2, _ = A([128, 4, 4], tok=t_k)
        gs, _ = A([128, 4], tok=t_k)
        gm, _ = A([128, 1], tok=t_k)
        sr, _ = A([128, 4], tok=t_k)
        w1, _ = A([128, 4], tok=t_k)
        w2, _ = A([128, 4], tok=t_k)
        cb = comb[R, j, :].rearrange("p (g e) -> p g e", g=4)
        tc_ = T_comb[j]
        V = lambda fn, rd=(), wr_=(): S.op("dve", fn, reads=[t_k] + list(rd), writes=[t_k] + list(wr_))
        V(lambda e: e.tensor_reduce(out=mx[R, :], in_=ps[R, 0:16], axis=AX.X, op=ALU.max), rd=[tp])
        V(lambda e: e.tensor_scalar(out=mx[R, :], in0=mx[R, :], scalar1=-1.0, scalar2=None, op0=ALU.mult))
        S.op("act", lambda e: e.activation(out=ee[R].rearrange("p g e -> p (g e)"), in_=ps[R, 0:16], func=AF.Exp,
                                           bias=mx[R, :], scale=1.0), reads=[tp, t_k], writes=[t_k])
        V(lambda e: e.tensor_reduce(out=m1[R, :], in_=ee[R], axis=AX.X, op=ALU.max))
        V(lambda e: e.tensor_tensor(out=eq1[R], in0=ee[R], in1=bc(m1[R, :], 2, [rows, 4, 4]),
                                    op=ALU.is_equal))
        V(lambda e: e.scalar_tensor_tensor(out=e2[R], in0=eq1[R], scalar=-4.0, in1=ee[R], op0=ALU.mult, op1=ALU.add))
        V(lambda e: e.tensor_reduce(out=m2[R, :], in_=e2[R], axis=AX.X, op=ALU.max))
        V(lambda e: e.tensor_tensor(out=eq2[R], in0=e2[R], in1=bc(m2[R, :], 2, [rows, 4, 4]),
                                    op=ALU.is_equal))
        V(lambda e: e.tensor_tensor(out=gs[R, :], in0=m1[R, :], in1=m2[R, :], op=ALU.add))
        V(lambda e: e.tensor_reduce(out=gm[R, :], in_=gs[R, :], axis=AX.X, op=ALU.max))
        V(lambda e: e.tensor_scalar(out=sr[R, :], in0=gs[R, :], scalar1=gm[R, :], scalar2=None, op0=ALU.is_equal))
        V(lambda e: e.reciprocal(out=gm[R, :], in_=gm[R, :]))
        V(lambda e: e.tensor_scalar(out=sr[R, :], in0=sr[R, :], scalar1=gm[R, :], scalar2=None, op0=ALU.mult))
        V(lambda e: e.tensor_tensor(out=w1[R, :], in0=m1[R, :], in1=sr[R, :], op=ALU.mult))
        V(lambda e: e.tensor_tensor(out=w2[R, :], in0=m2[R, :], in1=sr[R, :], op=ALU.mult))
        V(lambda e: e.tensor_tensor(out=eq1[R], in0=eq1[R], in1=bc(w1[R, :], 2, [rows, 4, 4]), op=ALU.mult))
        V(lambda e: e.tensor_tensor(out=eq2[R], in0=eq2[R], in1=bc(w2[R, :], 2, [rows, 4, 4]), op=ALU.mult))
        V(lambda e: e.tensor_tensor(out=cb, in0=eq1[R], in1=eq2[R], op=ALU.add), wr_=[tc_])

    def mixer(l, ti, subs, btok0, first_tile):
        slot0, T = tiles[ti]
        nch = T // CH
        tsl = slice(btok0, btok0 + T)
        T_hTt = [T_hT[j] for j, _, _ in subs]
        cw = convw[:, l]

        A_reset()
        BT, t_BT = A([128, 8, TT], BF16)
        CT, t_CT = A([128, 8, TT], BF16)
        pre = [A([128, 3 + TT]) for _ in range(3)]
        cacc = [A([128, TT]) for _ in range(3)]
        wv_dt, tw_dt = w_in_piece(l, COL_DT, 32)
        dtT, t_dt = A([32, TT])
        dAT, _ = A([32, TT], tok=t_dt)
        acsT, _ = A([32, TT], tok=t_dt)
        sdtT, _ = A([32, TT], tok=t_dt)
        tmpT, _ = A([32, TT], tok=t_dt)
        onesT, _ = A([32, CH], tok=t_dt)
        hiT, t_hl = A([32, TT], BF16)
        loT, _ = A([32, TT], BF16, tok=t_hl)

        def dt_chain():
            ps, tp = PS()
            for kc in range(8):
                S.op("pe", lambda e, kc=kc: e.matmul(ps[0:32, 0:T], lhsT=wv_dt[:, kc, 0:32], rhs=hT[:, kc, tsl],
                                                     start=(kc == 0), stop=(kc == 7)), reads=[tw_dt] + T_hTt, writes=[tp])
            yield
            S.op("act", lambda e: e.activation(out=tmpT[:, 0:T], in_=ps[0:32, 0:T], func=AF.Exp,
                                               bias=vecs[0:32, l, 56:57], scale=1.0), reads=[tp, T_cst], writes=[t_dt])
            S.op("act", lambda e: e.activation(out=dtT[:, 0:T], in_=tmpT[:, 0:T], func=AF.Ln, bias=epsc[0:32, 2:3], scale=1.0),
                 reads=[t_dt], writes=[t_dt])
            yield
            if ti == 0:
                S.op("dve", lambda e: e.memset(dtT[:, 0:48], 0.0), reads=[t_dt], writes=[t_dt])
            S.op("dve", lambda e: e.tensor_scalar(out=dAT[:, 0:T], in0=dtT[:, 0:T], scalar1=Aneg[:, l:l + 1], scalar2=None,
                                                  op0=ALU.mult), reads=[t_dt, T_cst], writes=[t_dt])
            S.op("dve", lambda e: e.memset(onesT[:], 1.0), writes=[t_dt])
            for c in range(nch):
                cs = slice(c * CH, (c + 1) * CH)
                S.op("dve", lambda e, cs=cs: e.tensor_tensor_scan(out=acsT[:, cs], data0=onesT[:], data1=dAT[:, cs],
                                                                  initial=0.0, op0=ALU.mult, op1=ALU.add),
                     reads=[t_dt], writes=[t_dt])
            yield
            for c in range(nch):
                cs = slice(c * CH, (c + 1) * CH)
                S.op("act", lambda e, cs=cs, c=c: e.activation(out=tmpT[:, cs], in_=acsT[:, cs], func=AF.Exp,
                                                               bias=acsT[:, c * CH + 63:c * CH + 64], scale=-1.0),
                     reads=[t_dt], writes=[t_dt])
            S.op("act", lambda e: e.activation(out=hiT[:, 0:T], in_=acsT[:, 0:T], func=AF.Copy), reads=[t_dt], writes=[t_hl])
            yield
            S.op("dve", lambda e: e.tensor_tensor(out=sdtT[:, 0:T], in0=dtT[:, 0:T], in1=tmpT[:, 0:T], op=ALU.mult),
                 reads=[t_dt], writes=[t_dt])
            S.op("dve", lambda e: e.tensor_tensor(out=loT[:, 0:T], in0=acsT[:, 0:T], in1=hiT[:, 0:T], op=ALU.subtract),
                 reads=[t_dt, t_hl], writes=[t_hl])
            yield

        dtg = dt_chain()
        dt_steps = {1: 1, 5: 1, 9: 1, 13: 1, 17: 1} if T == TT else {}
        def hist_in(b):
            pbn, t_pbn = pre[b % 3]
            S.op("dve", lambda e: e.tensor_copy(out=pbn[:, 0:3], in_=chist[l][:, b, :]),
                 reads=[T_chist[l]], writes=[t_pbn])

        hist_in(0)
        pend_silu = None
        for pi in range(8):
            wv, tw = w_in_piece(l, COL_X + pi * 512, 512, idx=pi)
            for cbk in range(4):
                b = pi * 4 + cbk
                ps, tp = PS()
                for kc in range(8):
                    S.op("pe", lambda e, kc=kc, cbk=cbk: e.matmul(ps[:, 0:T], lhsT=wv[:, kc, cbk * 128:(cbk + 1) * 128],
                                                                   rhs=hT[:, kc, tsl], start=(kc == 0), stop=(kc == 7)),
                         reads=[tw] + T_hTt, writes=[tp])
                pb, t_pb = pre[b % 3]
                ca, t_ca = cacc[b % 3]
                S.op("act", lambda e: e.activation(out=pb[:, 3:3 + T], in_=ps[:, 0:T], func=AF.Copy),
                     reads=[tp], writes=[t_pb])
                S.op("dve", lambda e, b=b: e.tensor_copy(out=chist[l][:, b, :], in_=pb[:, T:T + 3]),
                     reads=[t_pb], writes=[T_chist[l]])
                S.op("act", lambda e, b=b: e.activation(out=ca[:, 0:T], in_=pb[:, 0:T], func=AF.Identity,
                                                        bias=cw[:, b, 4:5], scale=cw[:, b, 0:1]),
                     reads=[t_pb, T_cst], writes=[t_ca])
                if pend_silu is not None:
                    pend_silu()
                if b + 1 < 32:
                    hist_in(b + 1)
                for jj in range(1, 4):
                    S.op("dve", lambda e, b=b, jj=jj: e.scalar_tensor_tensor(out=ca[:, 0:T], in0=pb[:, jj:jj + T],
                                                                             scalar=cw[:, b, jj:jj + 1], in1=ca[:, 0:T],
                                                                             op0=ALU.mult, op1=ALU.add),
                         reads=[t_pb, t_ca, T_cst], writes=[t_ca])
                if b < 16:
                    dst, t_dst = xT[:, b, 0:T], T_xT
                elif b < 24:
                    dst, t_dst = BT[:, b - 16, 0:T], t_BT
                else:
                    dst, t_dst = CT[:, b - 24, 0:T], t_CT

                def pend_silu(dst=dst, t_dst=t_dst, ca=ca, t_ca=t_ca):
                    S.op("act", lambda e: e.activation(out=dst, in_=ca[:, 0:T], func=AF.Silu),
                         reads=[t_ca], writes=[t_dst])
                for _ in range(dt_steps.get(b, 0)):
                    next(dtg, None)
        pend_silu()
        for _ in dtg:
            pass

        arow, t_arow = A([2, 2048], BF16)
        tok4, t_tok = A([64, 4, 32])
        nhi, t_n = A([64, 32], BF16)
        nlo, _ = A([64, 32], BF16, tok=t_n)
        eab = [A([64, 32]) for _ in range(2)]
        cdb = [A([128, 32]) for _ in range(2)]
        xs, t_xs = A([64, 32, 64], BF16)
        xsd, t_xsd = A([64, 32, 64], BF16)
        Btok, t_Btok = A([64, 8, 128], BF16)
        Ebb = [A([64, 16, 64], BF16) for _ in range(2)]
        MTb = [A([64, 16, 64], BF16) for _ in range(2)]
        t1, t_t1 = A([64, 16, 64])
        ybf, t_ybf = A([64, 16, 64], BF16)
        cbs, t_cbs = A([64, 8, 64])

        def stage_P(c):
            cs = slice(c * CH, (c + 1) * CH)
            ea, t_ea = eab[c % 2]
            cd, t_cd = cdb[c % 2]
            S.dma("sp", arow[0:1, :], hiT[:, cs], D_row, reads=[t_hl], writes=[t_arow])
            S.dma("sp", arow[1:2, :], loT[:, cs], D_row, reads=[t_hl], writes=[t_arow])
            ps, tp = PS()
            for q, src in enumerate((dtT, acsT, sdtT, dAT)):
                S.op("pe", lambda e, q=q, src=src, ps=ps: e.transpose(out=ps[0:64, q * 32:(q + 1) * 32], in_=src[:, cs],
                                                                      identity=ident32[0:32, 0:32]),
                     reads=[t_dt, T_cst], writes=[tp])
            S.op("act", lambda e, ps=ps: e.activation(out=tok4[:].rearrange("p a b -> p (a b)"), in_=ps[0:64, 0:128], func=AF.Copy),
                 reads=[tp], writes=[t_tok])
            S.op("act", lambda e: e.activation(out=ea[:], in_=tok4[:, 1, :], func=AF.Exp), reads=[t_tok], writes=[t_ea])
            S.op("dve", lambda e: e.tensor_scalar(out=nhi[:], in0=tok4[:, 1, :], scalar1=-1.0, scalar2=None, op0=ALU.mult),
                 reads=[t_tok], writes=[t_n])
            S.op("dve", lambda e: e.scalar_tensor_tensor(out=nlo[:], in0=tok4[:, 1, :], scalar=-1.0, in1=nhi[:],
                                                         op0=ALU.mult, op1=ALU.subtract), reads=[t_tok], writes=[t_n])
            ps2, tp2 = PS()
            S.op("pe", lambda e: e.matmul(ps2[:, 0:32], lhsT=ones32[0:64, :], rhs=tok4[:, 3, :], start=True, stop=True),
                 reads=[t_tok, T_cst], writes=[tp2])
            S.op("act", lambda e: e.activation(out=cd[:], in_=ps2[:, 0:32], func=AF.Exp), reads=[tp2], writes=[t_cd])

        def stage_X(c):
            cs = slice(c * CH, (c + 1) * CH)
            for hh in range(2):
                ps, tp = PS()
                psb = ps[:, :].bitcast(BF16)
                for bb in range(8):
                    b = hh * 8 + bb
                    S.op("pe", lambda e, b=b, bb=bb, psb=psb: e.transpose(out=psb[0:64, bb * 128:(bb + 1) * 128],
                                                                          in_=xT[:, b, cs], identity=identb[:, :]),
                         reads=[T_xT, T_cst], writes=[tp])
                pv = psb[0:64, :].rearrange("p (h d) -> p h d", h=16)
                hs = slice(hh * 16, hh * 16 + 16)
                S.op("dve", lambda e, pv=pv, hs=hs: e.tensor_tensor(out=xs[:, hs, :], in0=pv,
                                                                    in1=bc(tok4[:, 0, hs], 2, [64, 16, 64]),
                                                                    op=ALU.mult), reads=[tp, t_tok], writes=[t_xs])
                S.op("dve", lambda e, pv=pv, hs=hs: e.tensor_tensor(out=xsd[:, hs, :], in0=pv,
                                                                    in1=bc(tok4[:, 2, hs], 2, [64, 16, 64]),
                                                                    op=ALU.mult), reads=[tp, t_tok], writes=[t_xsd])
            ps, tp = PS()
            psb = ps[:, :].bitcast(BF16)
            for g in range(8):
                S.op("pe", lambda e, g=g, psb=psb: e.transpose(out=psb[0:64, g * 128:(g + 1) * 128], in_=BT[:, g, cs],
                                                               identity=identb[:, :]), reads=[t_BT, T_cst], writes=[tp])
            S.op("act", lambda e, psb=psb: e.activation(out=Btok[:].rearrange("p g n -> p (g n)"), in_=psb[0:64, :],
                                                        func=AF.Copy), reads=[tp], writes=[t_Btok])
            psCB, tCB = PS()
            for g in range(8):
                S.op("pe", lambda e, g=g, psCB=psCB: e.matmul(psCB[0:64, g * 64:(g + 1) * 64], lhsT=BT[:, g, cs], rhs=CT[:, g, cs],
                                                              start=True, stop=True), reads=[t_BT, t_CT], writes=[tCB])
            S.op("act", lambda e, psCB=psCB: e.activation(out=cbs[:].rearrange("p g l -> p (g l)"), in_=psCB[0:64, :], func=AF.Copy),
                 reads=[tCB], writes=[t_cbs])

        def stage_S(c):
            for hh in range(2):
                Eb, t_E = Ebb[hh]
                MT, t_MT = MTb[hh]
                for bk in range(2):
                    ps, tp = PS()
                    h0 = hh * 16 + bk * 8
                    outv = ps[0:64, :].rearrange("p (h l) -> p h l", h=8)
                    mm = [
                        (onesb[0:2, 0:64], arow[0:2, h0 * 64:(h0 + 8) * 64].rearrange("p (h l) -> p h l", h=8), [t_arow]),
                        (identb[0:64, 0:64], bc(nhi[:, h0:h0 + 8], 2, [64, 8, 64]), [t_n]),
                        (identb[0:64, 0:64], bc(nlo[:, h0:h0 + 8], 2, [64, 8, 64]), [t_n]),
                        (identb[0:64, 0:64], bc(negmb[:, :], 1, [64, 8, 64]), []),
                    ]
                    for mi, (lt, rh, rd) in enumerate(mm):
                        S.op("pe", lambda e, lt=lt, rh=rh, mi=mi, outv=outv: e.matmul(outv, lhsT=lt, rhs=rh,
                                                                                       start=(mi == 0), stop=(mi == 3)),
                             reads=[T_cst] + rd, writes=[tp])
                    S.op("act", lambda e, ps=ps, bk=bk, Eb=Eb: e.activation(
                        out=Eb[:, bk * 8:(bk + 1) * 8, :].rearrange("p h l -> p (h l)"), in_=ps[0:64, :], func=AF.Exp),
                         reads=[tp], writes=[t_E])
                g0 = hh * 4
                S.op("dve", lambda e, g0=g0, MT=MT, Eb=Eb: e.tensor_tensor(
                    out=MT[:].rearrange("p (g r) l -> p g r l", g=4),
                    in0=Eb[:].rearrange("p (g r) l -> p g r l", g=4),
                    in1=bc(cbs[:, g0:g0 + 4, :], 2, [64, 4, 4, 64]), op=ALU.mult),
                     reads=[t_E, t_cbs], writes=[t_MT])

        def stage_Y(c):
            cs = slice(c * CH, (c + 1) * CH)
            ea, t_ea = eab[c % 2]
            psO, psY = {}, {}
            for hh in range(2):
                psO[hh] = [PS(), PS()]
                for gl in range(4):
                    g = hh * 4 + gl
                    pO, tO = psO[hh][gl // 2]
                    S.op("pe", lambda e, pO=pO, gl=gl, g=g: e.matmul(pO[0:64, (gl % 2) * 256:(gl % 2 + 1) * 256],
                                                                     lhsT=CT[:, g, cs], rhs=stb[l][:, g * 256:(g + 1) * 256],
                                                                     start=True, stop=True),
                         reads=[t_CT, T_stb[l]], writes=[tO])
            for hh in range(2):
                MT, t_MT = MTb[hh]
                psY[hh] = [PS(), PS()]
                for hl in range(16):
                    h = hh * 16 + hl
                    pY, tY = psY[hh][hl // 8]
                    S.op("pe", lambda e, pY=pY, hl=hl, h=h, MT=MT: e.matmul(pY[0:64, (hl % 8) * 64:(hl % 8 + 1) * 64],
                                                                            lhsT=MT[:, hl, :], rhs=xs[:, h, :], start=True, stop=True),
                         reads=[t_MT, t_xs], writes=[tY])
            for hh in range(2):
                for bk in range(2):
                    h0 = hh * 16 + bk * 8
                    pO, tO = psO[hh][bk]
                    pY, tY = psY[hh][bk]
                    S.op("dve", lambda e, pO=pO, bk=bk, h0=h0: e.tensor_tensor(
                        out=t1[:, bk * 8:(bk + 1) * 8, :], in0=pO[0:64, :].rearrange("p (h d) -> p h d", h=8),
                        in1=bc(ea[:, h0:h0 + 8], 2, [64, 8, 64]), op=ALU.mult),
                         reads=[tO, t_ea], writes=[t_t1])
                    S.op("dve", lambda e, pY=pY, bk=bk: e.tensor_tensor(
                        out=ybf[:, bk * 8:(bk + 1) * 8, :], in0=pY[0:64, :].rearrange("p (h d) -> p h d", h=8),
                        in1=t1[:, bk * 8:(bk + 1) * 8, :], op=ALU.add), reads=[tY, t_t1], writes=[t_ybf])
                ps, tp = PS()
                psb = ps[:, :].bitcast(BF16)
                for bb in range(8):
                    S.op("pe", lambda e, bb=bb, psb=psb: e.transpose(out=psb[:, bb * 64:(bb + 1) * 64],
                                                                     in_=ybf[:, 2 * bb:2 * bb + 2, :].rearrange("p h d -> p (h d)"),
                                                                     identity=identb[0:64, 0:64]), reads=[t_ybf, T_cst], writes=[tp])
                S.op("act", lambda e, psb=psb, hh=hh: e.activation(out=yT[:, hh * 8:hh * 8 + 8, cs],
                                                                   in_=psb[:, 0:512].rearrange("p (b l) -> p b l", b=8),
                                                                   func=AF.Copy), reads=[tp], writes=[T_yT])

        def stage_U(c):
            cd, t_cd = cdb[c % 2]
            psS = {}
            for hh in range(2):
                psS[hh] = [PS(), PS()]
                for gl in range(4):
                    g = hh * 4 + gl
                    pS, tS = psS[hh][gl // 2]
                    S.op("pe", lambda e, pS=pS, gl=gl, g=g: e.matmul(pS[:, (gl % 2) * 256:(gl % 2 + 1) * 256],
                                                                     lhsT=Btok[:, g, :],
                                                                     rhs=xsd[:, 4 * g:4 * g + 4, :].rearrange("p h d -> p (h d)"),
                                                                     start=True, stop=True),
                         reads=[t_Btok, t_xsd], writes=[tS])
            for hh in range(2):
                for bk in range(2):
                    h0 = hh * 16 + bk * 8
                    pS, tS = psS[hh][bk]
                    sv = st32[l][:, h0 * 64:(h0 + 8) * 64].rearrange("p (h d) -> p h d", h=8)
                    S.op("dve", lambda e, sv=sv, h0=h0: e.tensor_tensor(out=sv, in0=sv,
                                                                        in1=bc(cd[:, h0:h0 + 8], 2, [128, 8, 64]),
                                                                        op=ALU.mult), reads=[t_cd, T_st32[l]], writes=[T_st32[l]])
                    S.op("dve", lambda e, sv=sv, pS=pS: e.tensor_tensor(out=sv, in0=sv,
                                                                        in1=pS[:, :].rearrange("p (h d) -> p h d", h=8),
                                                                        op=ALU.add), reads=[tS, T_st32[l]], writes=[T_st32[l]])
            S.op("act", lambda e: e.activation(out=stb[l][:], in_=st32[l][:], func=AF.Copy),
                 reads=[T_st32[l]], writes=[T_stb[l]])

        stage_P(0)
        for c in range(nch):
            stage_X(c)
            stage_S(c)
            if c + 1 < nch:
                stage_P(c + 1)
            stage_Y(c)
            stage_U(c)

        if upto == "p1" and dbg:
            dump(tok4[:].rearrange("p a b -> p (a b)"), t_tok, 64, 128)
            dump(st32[l][:, 0:512], T_st32[l], 128, 512)
            dump16(yT[:, :, 0:64], T_yT, 128, 1024)
            dump16(xT[:, :, 0:64], T_xT, 128, 1024)
            raise _Stop()
        A_reset()
        szb = [A([128, TT]) for _ in range(4)]
        ydb = [A([128, TT]) for _ in range(4)]
        sqb = [A([128, TT], BF16) for _ in range(4)]
        vnw = vecs[:, l, 0:16]
        vdsk = vecs[:, l, 16:32]
        vpsc = vecs[:, l, 32:40]
        vbg = vecs[:, l, 40:56]
        ms_all, t_ms = A([128, 8, TT])
        pend_post = [None]

        def z_post(b, sq, t_sq, psn, tn):
            S.op("act", lambda e: e.activation(out=sq[:, 0:T], in_=yT[:, b, 0:T], func=AF.Square),
                 reads=[T_yT], writes=[t_sq])
            S.op("pe", lambda e: e.matmul(psn[:, 0:T], lhsT=onesb[:, :], rhs=sq[:, 0:T],
                                          start=(b % 2 == 0), stop=(b % 2 == 1)),
                 reads=[t_sq, T_cst], writes=[tn])
            if b % 2 == 1:
                S.op("dve", lambda e: e.tensor_scalar(out=ms_all[:, b // 2, 0:T], in0=psn[:, 0:T], scalar1=1.0 / 256.0,
                                                      scalar2=RMS_EPS, op0=ALU.mult, op1=ALU.add),
                     reads=[tn], writes=[t_ms])

        psn_cur = None
        for pi in range(4):
            wv, tw = w_in_piece(l, COL_Z + pi * 512, 512, idx=8 + pi)
            for cbk in range(4):
                b = pi * 4 + cbk
                if b % 2 == 0:
                    psn_cur = PS()
                ps, tp = PS()
                for kc in range(8):
                    S.op("pe", lambda e, kc=kc, cbk=cbk, ps=ps: e.matmul(ps[:, 0:T], lhsT=wv[:, kc, cbk * 128:(cbk + 1) * 128],
                                                                         rhs=hT[:, kc, tsl], start=(kc == 0), stop=(kc == 7)),
                         reads=[tw] + T_hTt, writes=[tp])
                sz, t_sz = szb[b % 4]
                yd, t_yd = ydb[b % 4]
                sq, t_sq = sqb[b % 4]
                S.op("dve", lambda e, b=b, yd=yd: e.scalar_tensor_tensor(out=yd[:, 0:T], in0=xT[:, b, 0:T],
                                                                         scalar=vdsk[:, b:b + 1], in1=yT[:, b, 0:T],
                                                                         op0=ALU.mult, op1=ALU.add),
                     reads=[T_xT, T_yT, T_cst], writes=[t_yd])
                S.op("act", lambda e, ps=ps, sz=sz: e.activation(out=sz[:, 0:T], in_=ps[:, 0:T], func=AF.Silu),
                     reads=[tp], writes=[t_sz])
                if pend_post[0] is not None:
                    pend_post[0]()
                S.op("dve", lambda e, b=b, yd=yd, sz=sz: e.tensor_tensor(out=yT[:, b, 0:T], in0=yd[:, 0:T], in1=sz[:, 0:T],
                                                                         op=ALU.mult), reads=[t_yd, t_sz], writes=[T_yT])
                pend_post[0] = (lambda b=b, sq=sq, t_sq=t_sq, pc=psn_cur: z_post(b, sq, t_sq, pc[0], pc[1]))
        pend_post[0]()
        msf = ms_all[:, :, :].rearrange("p g t -> p (g t)")
        S.op("act", lambda e: e.activation(out=msf, in_=msf, func=AF.Ln), reads=[t_ms], writes=[t_ms])
        S.op("act", lambda e: e.activation(out=msf, in_=msf, func=AF.Exp, scale=-0.5), reads=[t_ms], writes=[t_ms])
        for b in range(16):
            S.op("dve", lambda e, b=b: e.scalar_tensor_tensor(out=yT[:, b, 0:T], in0=yT[:, b, 0:T],
                                                              scalar=vnw[:, b:b + 1], in1=ms_all[:, b // 2, 0:T],
                                                              op0=ALU.mult, op1=ALU.mult),
                 reads=[T_yT, t_ms, T_cst], writes=[T_yT])
        if upto == "p2a":
            dump16(yT[:, :, 0:64], T_yT, 128, 1024)
            raise _Stop()
        mixp, t_mixp = A([128, 8, TT], BF16)
        ppb = [A([128, 15 + TT]) for _ in range(2)]
        pwa = [A([128, 15 + TT]) for _ in range(2)]
        pwb = [A([128, 15 + TT]) for _ in range(2)]
        for pi in range(2):
            wv, tw = w_in_piece(l, COL_POOL + pi * 512, 512, idx=12 + pi)
            for cbk in range(4):
                b = pi * 4 + cbk
                g = b // 2
                ps, tp = PS()
                for kc in range(8):
                    S.op("pe", lambda e, kc=kc, cbk=cbk, ps=ps: e.matmul(ps[:, 0:T], lhsT=wv[:, kc, cbk * 128:(cbk + 1) * 128],
                                                                         rhs=hT[:, kc, tsl], start=(kc == 0), stop=(kc == 7)),
                         reads=[tw] + T_hTt, writes=[tp])
                pp, t_pp = ppb[b % 2]
                wa, t_wa = pwa[b % 2]
                wb, t_wb = pwb[b % 2]
                S.op("dve", lambda e, b=b, pp=pp: e.tensor_copy(out=pp[:, 0:15], in_=phist[l][:, b, :]),
                     reads=[T_phist[l]], writes=[t_pp])
                S.op("act", lambda e, pp=pp, ps=ps: e.activation(out=pp[:, 15:15 + T], in_=ps[:, 0:T], func=AF.Copy),
                     reads=[tp], writes=[t_pp])
                S.op("dve", lambda e, b=b, pp=pp: e.tensor_copy(out=phist[l][:, b, :], in_=pp[:, T:T + 15]),
                     reads=[t_pp], writes=[T_phist[l]])
                src, t_src = pp, t_pp
                bufs = [(wa, t_wa), (wb, t_wb)]
                for si in range(g + 1):
                    sh = 1 << si
                    dstb, t_dstb = bufs[si % 2]
                    n = 15 + T - sh
                    S.op("dve", lambda e, src=src, dstb=dstb, sh=sh, n=n: e.tensor_tensor(
                        out=dstb[:, sh:sh + n], in0=src[:, sh:sh + n], in1=src[:, 0:n], op=ALU.add),
                         reads=[t_src], writes=[t_dstb])
                    src, t_src = dstb, t_dstb
                if ti == 0:
                    S.op("dve", lambda e, src=src, g=g: e.tensor_tensor(out=src[:, 15:15 + T], in0=src[:, 15:15 + T],
                                                                        in1=rc0[:, g, 0:T], op=ALU.mult),
                         reads=[t_src, T_cst], writes=[t_src])
                    S.op("dve", lambda e, src=src, b=b, pp=pp: e.tensor_tensor(out=mixp[:, b, 0:T], in0=src[:, 15:15 + T],
                                                                               in1=pp[:, 15:15 + T], op=ALU.subtract),
                         reads=[t_src, t_pp], writes=[t_mixp])
                else:
                    S.op("dve", lambda e, src=src, b=b, pp=pp, g=g: e.scalar_tensor_tensor(
                        out=mixp[:, b, 0:T], in0=src[:, 15:15 + T], scalar=1.0 / POOLW[g], in1=pp[:, 15:15 + T],
                        op0=ALU.mult, op1=ALU.subtract), reads=[t_src, t_pp], writes=[t_mixp])
        if upto == "p2b":
            raise _Stop()
        mixT, t_mixT = A([128, 8, TT], BF16)
        g0b = [A([128, TT]) for _ in range(2)]
        g1b = [A([128, TT]) for _ in range(2)]
        wplv = wpool_sb[:, l, :].rearrange("p (g c d) -> p g c d", g=4, c=2)
        twpl = T_cst
        w_in_v = w_in[l].rearrange("(kc p) c -> p kc c", p=128)
        w_so_v = w_so[l].rearrange("(i p) d -> p i d", p=128)
        for db in range(8):
            c0 = COL_GATE + db * 128
            pc, tws = wpiece(l, 14 + db, [
                (lambda r: r[:, 0:1024].rearrange("p (kc c) -> p kc c", kc=8), w_in_v[:, :, c0:c0 + 128]),
                (lambda r: r[:, 1024:2048].rearrange("p (kc c) -> p kc c", kc=8), w_in_v[:, :, c0 + 1024:c0 + 1024 + 128]),
                (lambda r: r[:, 2048:4096].rearrange("p (i d) -> p i d", i=16), w_so_v[:, :, db * 128:(db + 1) * 128]),
            ])
            wg0 = pc[:, 0:1024].rearrange("p (kc c) -> p kc c", kc=8)
            wg1 = pc[:, 1024:2048].rearrange("p (kc c) -> p kc c", kc=8)
            wsv = pc[:, 2048:4096].rearrange("p (i d) -> p i d", i=16)
            g0, t_g0 = g0b[db % 2]
            g1, t_g1 = g1b[db % 2]
            for (wv, gg, t_gg, bcol) in ((wg0, g0, t_g0, db), (wg1, g1, t_g1, 8 + db)):
                ps, tp = PS()
                for kc in range(8):
                    S.op("pe", lambda e, kc=kc, ps=ps, wv=wv: e.matmul(ps[:, 0:T], lhsT=wv[:, kc, :],
                                                                       rhs=hT[:, kc, tsl], start=(kc == 0), stop=(kc == 7)),
                         reads=[tws] + T_hTt, writes=[tp])
                S.op("act", lambda e, ps=ps, gg=gg, bcol=bcol: e.activation(out=gg[:, 0:T], in_=ps[:, 0:T], func=AF.Sigmoid,
                                                                            bias=vbg[:, bcol:bcol + 1], scale=1.0),
                     reads=[tp, T_cst], writes=[t_gg])
            psy, tpy = PS()
            for i in range(16):
                S.op("pe", lambda e, i=i, psy=psy, wsv=wsv: e.matmul(psy[:, 0:T], lhsT=wsv[:, i, :],
                                                                     rhs=yT[:, i, 0:T], start=(i == 0), stop=(i == 15)),
                     reads=[tws, T_yT], writes=[tpy])
            psp, tpp = PS()
            gp = db // 2
            for c2 in range(2):
                S.op("pe", lambda e, c2=c2, psp=psp, gp=gp, db=db: e.matmul(psp[:, 0:T],
                                                                            lhsT=wplv[:, gp, c2, (db % 2) * 128:(db % 2 + 1) * 128],
                                                                            rhs=mixp[:, gp * 2 + c2, 0:T], start=(c2 == 0), stop=(c2 == 1)),
                     reads=[twpl, t_mixp], writes=[tpp])
            S.op("dve", lambda e, g0=g0, psy=psy: e.tensor_tensor(out=g0[:, 0:T], in0=g0[:, 0:T], in1=psy[:, 0:T], op=ALU.mult),
                 reads=[tpy, t_g0], writes=[t_g0])
            S.op("dve", lambda e, g1=g1, psp=psp, db=db: e.scalar_tensor_tensor(out=g1[:, 0:T], in0=psp[:, 0:T],
                                                                                scalar=vpsc[:, db:db + 1], in1=g1[:, 0:T],
                                                                                op0=ALU.mult, op1=ALU.mult),
                 reads=[tpp, t_g1, T_cst], writes=[t_g1])
            S.op("dve", lambda e, g0=g0, g1=g1, db=db: e.tensor_tensor(out=mixT[:, db, 0:T], in0=g0[:, 0:T], in1=g1[:, 0:T], op=ALU.add),
                 reads=[t_g0, t_g1], writes=[t_mixT])
        if upto == "p2c":
            dump16(mixT[:, :, 0:64], t_mixT, 128, 512)
            dump16(mixp[:, :, 0:64], t_mixp, 128, 512)
            for q_ in range(2):
                dump(g0b[q_][0][:, 0:64], g0b[q_][1], 128, 64)
                dump(g1b[q_][0][:, 0:64], g1b[q_][1], 128, 64)
            raise _Stop()
        wo = [wpiece(l, 22 + hf, [(lambda r: r[:, :].rearrange("p (k d) -> p k d", k=8),
                                   w_out[l].rearrange("(k p) d -> p k d", p=128)[:, :, hf * 512:(hf + 1) * 512])]) for hf in range(2)]
        if first_tile:
            load_lnt(0, 2 + 4 * l)
            load_lnt(1, 3 + 4 * l)
        for (j, rows, btok) in subs:
            tl = btok - btok0
            for hf in range(2):
                pc, tk = wo[hf]
                wov = pc[:, :].rearrange("p (k d) -> p k d", k=8)
                ps, tp = PS()
                for k in range(8):
                    S.op("pe", lambda e, k=k, ps=ps, wov=wov: e.matmul(ps[0:rows, :], lhsT=mixT[:, k, tl:tl + rows], rhs=wov[:, k, :],
                                                                       start=(k == 0), stop=(k == 7)),
                         reads=[tk, t_mixT], writes=[tp])
                hv = hres[0:rows, j, hf * 512:(hf + 1) * 512]
                S.op("dve", lambda e, hv=hv, ps=ps: e.scalar_tensor_tensor(out=hv, in0=hv, scalar=ALPHA, in1=ps[0:rows, :],
                                                                           op0=ALU.mult, op1=ALU.add),
                     reads=[tp, T_hres[j]], writes=[T_hres[j]])
            if upto == "p2e":
                continue
            layer_norm_inplace(j, rows)
            if upto == "p2f":
                continue
            transpose_to_hT(j, rows, btok, router=(upto != "p2d"), mask0=False)

    def moe(l, allsubs, ntok):
        A_reset()
        load_lnt(0, 4 + 4 * l)
        load_lnt(1, 5 + 4 * l)
        mt = []
        cur = []
        for s_ in allsubs:
            if cur and (s_[2] + s_[1] - cur[0][2]) > 512:
                mt.append(cur)
                cur = []
            cur.append(s_)
        if cur:
            mt.append(cur)
        hm, t_hm = A([128, 4, 512], BF16)
        sgb = [A([128, 512]) for _ in range(2)]
        for (j, rows, btok) in allsubs:
            S.op("act", lambda e, j=j, rows=rows: e.activation(out=hres[0:rows, j, :], in_=hres[0:rows, j, :], func=AF.Copy,
                                                               scale=ALPHA), reads=[T_hres[j]], writes=[T_hres[j]])
        for ex in range(NE):
            wg, twg = wload([(lambda r: r[:, :].rearrange("p (k f) -> p k f", k=8),
                              w_eg[l, ex].rearrange("(k p) f -> p k f", p=128))])
            wu, twu = wload([(lambda r: r[:, :].rearrange("p (k f) -> p k f", k=8),
                              w_eu[l, ex].rearrange("(k p) f -> p k f", p=128))])
            wd, twd = wload([(lambda r: r[:, :].rearrange("p (k d) -> p k d", k=4),
                              w_ed[l, ex].rearrange("(k p) d -> p k d", p=128))])
            wgv = wg[:, :].rearrange("p (k f) -> p k f", k=8)
            wuv = wu[:, :].rearrange("p (k f) -> p k f", k=8)
            wdv = wd[:, :].rearrange("p (k d) -> p k d", k=4)
            for grp in mt:
                b0 = grp[0][2]
                n = grp[-1][2] + grp[-1][1] - b0
                rd_hT = [T_hT[j] for j, _, _ in grp]
                for fb in range(4):
                    psg, tg = PS()
                    psu, tu = PS()
                    for kc in range(8):
                        S.op("pe", lambda e, kc=kc, fb=fb, psg=psg: e.matmul(psg[:, 0:n], lhsT=wgv[:, kc, fb * 128:(fb + 1) * 128],
                                                                             rhs=hT[:, kc, b0:b0 + n], start=(kc == 0), stop=(kc == 7)),
                             reads=[twg] + rd_hT, writes=[tg])
                    for kc in range(8):
                        S.op("pe", lambda e, kc=kc, fb=fb, psu=psu: e.matmul(psu[:, 0:n], lhsT=wuv[:, kc, fb * 128:(fb + 1) * 128],
                                                                             rhs=hT[:, kc, b0:b0 + n], start=(kc == 0), stop=(kc == 7)),
                             reads=[twu] + rd_hT, writes=[tu])
                    sg, t_sg = sgb[fb % 2]
                    S.op("act", lambda e, psg=psg, sg=sg: e.activation(out=sg[:, 0:n], in_=psg[:, 0:n], func=AF.Silu),
                         reads=[tg], writes=[t_sg])
                    S.op("dve", lambda e, psu=psu, sg=sg, fb=fb: e.tensor_tensor(out=hm[:, fb, 0:n], in0=sg[:, 0:n], in1=psu[:, 0:n],
                                                                                 op=ALU.mult), reads=[tu, t_sg], writes=[t_hm])
                for (j, rows, btok) in grp:
                    tl = btok - b0
                    for hf in range(2):
                        ps, tp = PS()
                        for fb in range(4):
                            S.op("pe", lambda e, fb=fb, ps=ps, tl=tl, rows=rows, hf=hf: e.matmul(
                                ps[0:rows, :], lhsT=hm[:, fb, tl:tl + rows], rhs=wdv[:, fb, hf * 512:(hf + 1) * 512],
                                start=(fb == 0), stop=(fb == 3)), reads=[t_hm, twd], writes=[tp])
                        hv = hres[0:rows, j, hf * 512:(hf + 1) * 512]
                        S.op("dve", lambda e, hv=hv, ps=ps, j=j, rows=rows, ex=ex: e.scalar_tensor_tensor(
                            out=hv, in0=ps[0:rows, :], scalar=comb[0:rows, j, ex:ex + 1], in1=hv, op0=ALU.mult, op1=ALU.add),
                             reads=[tp, T_hres[j], T_comb[j]], writes=[T_hres[j]])
        for (j, rows, btok) in allsubs:
            layer_norm_inplace(j, rows)
            if l + 1 < nlayers:
                transpose_to_hT(j, rows, btok, router=False, mask0=(rows == 64))

    for bi in range(nblocks):
        A_reset()
        tl_list = blocks[bi]
        allsubs = []
        tile_subs = []
        btok = 0
        j = 0
        for ti in tl_list:
            slot0, T = tiles[ti]
            subs = []
            nsub = max(1, T // 128)
            for s_ in range(nsub):
                rows = min(128, T)
                subs.append((j, rows, btok))
                S.dma("sp", hres[0:rows, j, :], xin[slot0 + s_ * 128: slot0 + s_ * 128 + rows, :], D_x[j], writes=[T_hres[j]])
                btok += rows
                j += 1
            tile_subs.append((ti, subs, subs[0][2]))
            allsubs += subs
        try:
            load_lnt(0, 0)
            load_lnt(1, 1)
            for (j, rows, bt) in allsubs:
                layer_norm_inplace(j, rows)
                transpose_to_hT(j, rows, bt, router=False, mask0=(rows == 64))
            if upto == "ln0":
                raise _Stop()
            for l in range(nlayers):
                for k, (ti, subs, bt0) in enumerate(tile_subs):
                    mixer(l, ti, subs, bt0, first_tile=(k == 0))
                    wsc_ready[l] = True
                    if upto in ("tile0", "p2d", "p2e", "p2f"):
                        raise _Stop()
                if upto == "mixer":
                    raise _Stop()
                moe(l, allsubs, btok)
        except _Stop:
            for (j, rows, bt) in allsubs:
                S.dma("sp", out_d[bt:bt + rows, :], hres[0:rows, j, :], D_out, reads=[T_hres[j]], writes=[T_out])
            break
        for (j, rows, bt) in allsubs:
            if rows == 64:
                continue
            orow = tiles[tl_list[0]][0] + bt - 64
            S.dma("sp", out_d[orow:orow + rows, :], hres[0:rows, j, :], D_out, reads=[T_hres[j]], writes=[T_out])
    S.wait_all("sp", [D_out, D_dbg] + D_ring)
    return nc


def _consts():
    c = np.zeros((128, 1024), np.float32)
    c[:, 0:128] = np.eye(128, dtype=np.float32)
    c[:, 128:256] = 1.0
    t = np.arange(64)
    c[0:64, 256:320] = (t[:, None] <= t[None, :]).astype(np.float32)
    c[0:64, 320:384] = np.where(t[None, :] < t[:, None], -30000.0, 0.0).astype(np.float32)
    rc = np.zeros((4, 64), np.float32)
    for g, w in enumerate(POOLW):
        for s in range(64):
            tt = s - 48
            rc[g, s] = 1.0 / w if tt < 0 else 1.0 / min(tt + 1, w)
    c[:, 384:640] = rc.reshape(1, 256)
    c[0:64, 640] = (t >= 48).astype(np.float32)
    c[:, 641] = LN_EPS
    c[:, 642] = RMS_EPS
    c[:, 643] = 1.0
    return c


_NC_CACHE = {}


def kernel(x, meta_tokens, ln_in_g, ln_in_b, w_router, w_in, conv_w, conv_b, dt_bias, a_log, d_skip, ssd_norm_w,
           w_ssd_out, w_pool, pool_scale, b_gate, w_out, ln1_g, ln1_b, w_exp_gate, w_exp_up, w_exp_down, ln2_g, ln2_b,
           _cfg=None):
    f = lambda a: np.ascontiguousarray(np.asarray(a, dtype=np.float32))
    x = f(x)
    cfg = _cfg or {}
    ncores = cfg.get("ncores", 4)
    key = tuple(sorted(cfg.items()))
    if key not in _NC_CACHE:
        _NC_CACHE[key] = build(**cfg)
    nc = _NC_CACHE[key]
    lnrows = [ln_in_g, ln_in_b]
    for l in range(2):
        lnrows += [ln1_g[l], ln1_b[l], ln2_g[l], ln2_b[l]]
    lnt = np.ascontiguousarray(np.broadcast_to(np.stack([f(r) for r in lnrows])[:, None, :], (10, 128, D)))
    convw = np.zeros((2, 128, 32, 5), np.float32)
    vecs = np.zeros((2, 128, 64), np.float32)
    cw, cb = f(conv_w), f(conv_b)
    for l in range(2):
        convw[l, :, :, 0:4] = cw[l].reshape(4, 32, 128).transpose(2, 1, 0)
        convw[l, :, :, 4] = cb[l].reshape(32, 128).T
        vecs[l, :, 0:16] = f(ssd_norm_w)[l].reshape(16, 128).T
        vecs[l, :, 16:32] = np.repeat(f(d_skip)[l], 64).reshape(16, 128).T
        vecs[l, :, 32:40] = f(pool_scale)[l].reshape(8, 128).T
        vecs[l, :, 40:56] = f(b_gate)[l].reshape(16, 128).T
        vecs[l, 0:32, 56] = f(dt_bias)[l]
        vecs[l, 0:32, 57] = f(a_log)[l]
    wr = np.ascontiguousarray(f(w_router).reshape(8, 128, 16).transpose(1, 0, 2))
    shared = {
        "w_in": f(w_in), "w_so": f(w_ssd_out), "w_pool": f(w_pool), "w_out": f(w_out),
        "w_eg": f(w_exp_gate), "w_eu": f(w_exp_up), "w_ed": f(w_exp_down),
        "lnt": lnt, "cst": _consts(), "convw": convw, "vecs": vecs, "wr": wr,
    }
    in_maps = []
    meta = f(meta_tokens)
    for c in range(ncores):
        b = c % 4
        xin = np.zeros((NSLOT, D), np.float32)
        xin[48:64] = meta
        xin[64:] = x[b]
        m = dict(shared)
        m["xin"] = xin
        in_maps.append(m)
    tr = bool(cfg.get("trace"))
    core_ids = [0, 1, 4, 5] if ncores == 4 else list(range(ncores))
    res = run_bass_kernel_spmd(nc, in_maps, core_ids=core_ids, **({"trace": True} if tr else {}))
    if tr:
        print("EXEC_TIME_NS", res.exec_time_ns)
    out = np.stack([np.asarray(res.results[b]["out"], dtype=np.float32) for b in range(min(4, ncores))], axis=0)
    if cfg.get("dbg"):
        kernel.last_dbg = [np.asarray(res.results[b]["dbg"]) for b in range(min(4, ncores))]
        kernel.last_dbgb = [np.asarray(res.results[b]["dbgb"]).astype(np.float32) for b in range(min(4, ncores))]
    return out
```

```python
import numpy as np
import concourse.bass as bass
import concourse.mybir as mybir
from concourse.bass_utils import run_bass_kernel_spmd

F32 = mybir.dt.float32
BF16 = mybir.dt.bfloat16
U8 = mybir.dt.uint8
AF = mybir.ActivationFunctionType
ALU = mybir.AluOpType
AX = mybir.AxisListType

D = 1024
NSLOT = 4160
CH = 64
TT = 256
NH = 32
NG = 8
COL_Z, COL_X, COL_B, COL_C, COL_DT, COL_POOL, COL_GATE, INC = 0, 2048, 4096, 5120, 6144, 6176, 7200, 9248
NE = 16
ALPHA = 4.0 ** 0.25
LN_EPS = 1e-5
RMS_EPS = 1e-5
NO_SELF_SYNC = False
NW = 4
ARENA = 55296
POOLW = (2, 4, 8, 16)


class Tok:
    __slots__ = ("w", "r", "name")

    def __init__(self, name=""):
        self.w = None
        self.r = {}
        self.name = name


class _Eng:
    def __init__(self, name, e, sem):
        self.name, self.e, self.sem, self.cnt, self.known = name, e, sem, 0, {}


class _DSem:
    def __init__(self, h):
        self.h, self.cnt = h, 0


class Sched:
    def __init__(self, nc):
        self.nc = nc
        self.nsem = 0
        self.engs = {}
        for name, e in (("pe", nc.tensor), ("act", nc.scalar), ("dve", nc.vector), ("pool", nc.gpsimd),
                        ("sp", nc.sync)):
            self.engs[name] = _Eng(name, e, self._sem("s_" + name))

    def _sem(self, name):
        self.nsem += 1
        return self.nc.semaphore(name).__enter__()

    def dsem(self, name):
        return _DSem(self._sem("d_" + name))

    def _wait(self, E, deps):
        best = {}
        for sem, val in deps:
            k = id(sem)
            if k not in best or best[k][1] < val:
                best[k] = (sem, val)
        for k, (sem, val) in best.items():
            if sem is E.sem and (E.name == "pe" or NO_SELF_SYNC):
                continue
            if E.known.get(k, 0) >= val:
                continue
            E.e.wait_ge(sem, val)
            E.known[k] = val

    @staticmethod
    def _deps(reads, writes):
        deps = []
        for t in reads:
            if t.w is not None:
                deps.append(t.w)
        for t in writes:
            if t.w is not None:
                deps.append(t.w)
            deps.extend(t.r.values())
        return deps

    def op(self, eng, fn, reads=(), writes=()):
        E = self.engs[eng]
        self._wait(E, self._deps(reads, writes))
        ins = fn(E.e)
        E.cnt += 1
        ins.then_inc(E.sem, 1)
        mark = (E.sem, E.cnt)
        for t in writes:
            t.w = mark
            t.r = {}
        for t in reads:
            if t.w is not mark:
                t.r[id(E.sem)] = mark

    def dma(self, queue, out, in_, ds, reads=(), writes=(), **kw):
        Q = self.engs[queue]
        self._wait(Q, self._deps(reads, writes))
        ins = Q.e.dma_start(out=out, in_=in_, **kw)
        ds.cnt += 16
        ins.then_inc(ds.h, 16)
        mark = (ds.h, ds.cnt)
        for t in writes:
            t.w = mark
            t.r = {}
        for t in reads:
            t.r[id(ds.h)] = mark

    def barrier(self, names=("pe", "act", "dve", "pool")):
        for a in names:
            A = self.engs[a]
            deps = [(self.engs[b].sem, self.engs[b].cnt) for b in names if b != a and self.engs[b].cnt > 0]
            self._wait(A, deps)

    def wait_all(self, eng, dsems):
        E = self.engs[eng]
        deps = [(x.sem, x.cnt) for x in self.engs.values() if x is not E and x.cnt > 0]
        deps += [(d.h, d.cnt) for d in dsems if d.cnt > 0]
        self._wait(E, deps)


def bc(ap, axis, shape):
    return ap.unsqueeze(axis).broadcast_to(list(shape))


def seq_tiles():
    tiles = [(0, 64)] + [(64 + i * TT, TT) for i in range((NSLOT - 64) // TT)]
    blocks = [[0, 1, 2, 3, 4]] + [list(range(5 + 4 * i, 9 + 4 * i)) for i in range(3)]
    return tiles, blocks


class _Stop(Exception):
    pass


def build(nblocks=4, nlayers=2, dbg=False, upto="full", ncores=8, trace=False):
    nc = bass.Bass("TRN2", target_bir_lowering=False)
    S = Sched(nc)

    def dram(name, shape, dt=F32, kind="ExternalInput"):
        return nc.dram_tensor(name, list(shape), dt, kind=kind).ap()

    xin = dram("xin", [NSLOT, D])
    w_in = dram("w_in", [2, D, INC])
    w_so = dram("w_so", [2, 2048, D])
    w_pool = dram("w_pool", [2, 4, 256, 256])
    w_out = dram("w_out", [2, D, D])
    w_eg = dram("w_eg", [2, NE, D, 512])
    w_eu = dram("w_eu", [2, NE, D, 512])
    w_ed = dram("w_ed", [2, NE, 512, D])
    lnt_d = dram("lnt", [10, 128, D])
    cst_d = dram("cst", [128, 1024])
    convw_d = dram("convw", [2, 128, 32, 5])
    vec_d = dram("vecs", [2, 128, 64])
    wr_d = dram("wr", [128, 8, 16])
    out_d = dram("out", [4096, D], kind="ExternalOutput")
    dbg_d = dram("dbg", [128, 8192], kind="ExternalOutput") if dbg else None
    dbgb_d = dram("dbgb", [128, 8192], BF16, kind="ExternalOutput") if dbg else None

    def sb(name, shape, dt=F32):
        return nc.alloc_sbuf_tensor(name, list(shape), dt)

    hres = sb("hres", [128, 9, D])
    hT = sb("hT", [128, 8, 1088], BF16)
    xT = sb("xT", [128, 16, TT], BF16)
    yT = sb("yT", [128, 16, TT], BF16)
    st32 = [sb(f"st32_{l}", [128, 2048]) for l in range(2)]
    stb = [sb(f"stb_{l}", [128, 2048], BF16) for l in range(2)]
    chist = [sb(f"chist_{l}", [128, 32, 3]) for l in range(2)]
    phist = [sb(f"phist_{l}", [128, 8, 15]) for l in range(2)]
    lnt = sb("lnt_sb", [128, 2, D])
    ring = [sb(f"ring{i}", [128, 4096], BF16) for i in range(NW)]
    cst = sb("cst_sb", [128, 1024])
    convw = sb("convw_sb", [128, 2, 32, 5])
    vecs = sb("vecs_sb", [128, 2, 64])
    wr = sb("wr_sb", [128, 8, 16])
    comb = sb("comb", [128, 9, 16])
    identb = sb("identb", [128, 128], BF16)
    onesb = sb("onesb", [128, 128], BF16)
    negmb = sb("negmb", [64, 64], BF16)
    Aneg = sb("Aneg", [32, 2])
    wrh = sb("wrh", [128, 8, 16], BF16)
    wrl = sb("wrl", [128, 8, 16], BF16)
    wpool_sb = sb("wpool_sb", [128, 2, 2048], BF16)
    arena = sb("arena", [128, ARENA], U8)

    T_hres = [Tok(f"hres{j}") for j in range(9)]
    T_hT = [Tok(f"hT{j}") for j in range(9)]
    T_xT, T_yT = Tok("xT"), Tok("yT")
    T_st32 = [Tok(), Tok()]
    T_stb = [Tok(), Tok()]
    T_chist = [Tok(), Tok()]
    T_phist = [Tok(), Tok()]
    T_lnt = [Tok(), Tok()]
    T_ring = [Tok(f"ring{i}") for i in range(NW)]
    T_cst = Tok("cst")
    T_comb = [Tok() for _ in range(9)]
    T_out = Tok("out")

    D_ring = [S.dsem(f"ring{i}") for i in range(NW)]
    D_in = S.dsem("in")
    D_x = [S.dsem(f"x{j}") for j in range(9)]
    D_lnt = [S.dsem("lnt0"), S.dsem("lnt1")]
    D_out = S.dsem("out")
    D_row = S.dsem("arow")
    D_dbg = S.dsem("dbg")

    ident32 = cst[:, 0:128]
    ones32 = cst[:, 128:256]
    tri32 = cst[0:64, 256:320]
    negm32 = cst[0:64, 320:384]
    rc0 = cst[:, 384:640].rearrange("p (g l) -> p g l", g=4)
    m0 = cst[0:64, 640:641]
    epsc = cst[:, 641:644]

    psum = [nc.alloc_psum_tensor(f"ps{i}", [128, 512], F32) for i in range(8)]
    T_ps = [Tok(f"ps{i}") for i in range(8)]
    ps_rr = [0]

    def PS():
        i = ps_rr[0]
        ps_rr[0] = (i + 1) % 8
        return psum[i], T_ps[i]

    aoff = [0]
    a_live = []
    a_old = []

    def A_reset(barrier=False):
        if barrier:
            S.barrier()
        a_old.extend(a_live)
        del a_live[:]
        aoff[0] = 0

    def A(shape, dt=F32, tok=None):
        esz = {F32: 4, BF16: 2}[dt]
        n = 1
        for s in shape[1:]:
            n *= s
        nb = (n * esz + 31) // 32 * 32
        o = aoff[0]
        aoff[0] += nb
        assert aoff[0] <= ARENA, f"arena overflow {aoff[0]}"
        v = arena[0:shape[0], o:o + n * esz].bitcast(dt)
        if len(shape) == 3:
            v = v.rearrange("p (a b) -> p a b", a=shape[1])
        elif len(shape) == 4:
            v = v.rearrange("p (a b c) -> p a b c", a=shape[1], b=shape[2])
        tk = tok if tok is not None else Tok()
        keep = []
        for (s0, e0, t0) in a_old:
            if e0 <= o or s0 >= o + nb:
                keep.append((s0, e0, t0))
                continue
            marks = list(t0.r.values())
            if t0.w is not None:
                marks.append(t0.w)
            for (sem, val) in marks:
                k = id(sem)
                if k not in tk.r or tk.r[k][1] < val:
                    tk.r[k] = (sem, val)
            if not (s0 >= o and e0 <= o + nb):
                keep.append((s0, e0, t0))
        a_old[:] = keep
        a_live.append((o, o + nb, tk))
        return v, tk

    dbg_col = [0]

    def dump(ap, tok, rows, ncols):
        if not dbg:
            return
        c = dbg_col[0]
        dbg_col[0] += ncols
        S.dma("sp", dbg_d[0:rows, c:c + ncols], ap, D_dbg, reads=[tok])
        return c

    dbgb_col = [0]

    def dump16(ap, tok, rows, ncols):
        if not dbg:
            return
        c = dbgb_col[0]
        dbgb_col[0] += ncols
        S.dma("sp", dbgb_d[0:rows, c:c + ncols], ap, D_dbg, reads=[tok])

    ring_i = [0]

    def wload(parts):
        i = ring_i[0]
        ring_i[0] = (i + 1) % NW
        for dst_fn, src in parts:
            S.dma("pool", dst_fn(ring[i]), src, D_ring[i], writes=[T_ring[i]])
        return ring[i], T_ring[i]

    NPC = 24
    wsc = dram("wsc", [2 * NPC, 128, 4096], BF16, kind="Internal")
    T_wsc = [Tok("wsc0"), Tok("wsc1")]
    D_wst = S.dsem("wst")
    wsc_ready = [False, False]

    def wpiece(l, idx, parts):
        if not wsc_ready[l]:
            pc, tk = wload(parts)
            S.dma("sp", wsc[l * NPC + idx], pc[:, :], D_wst, reads=[tk])
            T_wsc[l].w = (D_wst.h, D_wst.cnt)
            return pc, tk
        i = ring_i[0]
        ring_i[0] = (i + 1) % NW
        S.dma("pool", ring[i][:, :], wsc[l * NPC + idx], D_ring[i], reads=[T_wsc[l]], writes=[T_ring[i]])
        return ring[i], T_ring[i]

    def w_in_piece(l, c0, ncol, idx=None):
        src = w_in[l].rearrange("(kc p) c -> p kc c", p=128)[:, :, c0:c0 + ncol]
        parts = [(lambda r: r[:, 0:8 * ncol].rearrange("p (kc c) -> p kc c", kc=8), src)]
        pc, tk = wload(parts) if idx is None else wpiece(l, idx, parts)
        return pc[:, 0:8 * ncol].rearrange("p (kc c) -> p kc c", kc=8), tk

    S.dma("sp", cst[:], cst_d[:, :], D_in, writes=[T_cst])
    S.dma("sp", convw[:], convw_d.rearrange("l p b j -> p l b j"), D_in, writes=[T_cst])
    S.dma("sp", vecs[:], vec_d.rearrange("l p c -> p l c"), D_in, writes=[T_cst])
    S.dma("sp", wr[:], wr_d[:, :, :], D_in, writes=[T_cst])
    S.op("act", lambda e: e.activation(out=identb[:], in_=ident32, func=AF.Copy), reads=[T_cst], writes=[T_cst])
    S.op("act", lambda e: e.activation(out=onesb[:], in_=ones32, func=AF.Copy), reads=[T_cst], writes=[T_cst])
    S.op("act", lambda e: e.activation(out=negmb[:], in_=negm32, func=AF.Copy), reads=[T_cst], writes=[T_cst])
    S.op("act", lambda e: e.activation(out=wrh[:], in_=wr[:], func=AF.Copy), reads=[T_cst], writes=[T_cst])
    S.op("dve", lambda e: e.tensor_tensor(out=wrl[:], in0=wr[:], in1=wrh[:], op=ALU.subtract), reads=[T_cst], writes=[T_cst])
    S.op("act", lambda e: e.activation(out=Aneg[:, 0:1], in_=vecs[0:32, 0, 57:58], func=AF.Exp),
         reads=[T_cst], writes=[T_cst])
    S.op("act", lambda e: e.activation(out=Aneg[:, 1:2], in_=vecs[0:32, 1, 57:58], func=AF.Exp),
         reads=[T_cst], writes=[T_cst])
    S.op("dve", lambda e: e.tensor_scalar(out=Aneg[:], in0=Aneg[:], scalar1=-1.0, scalar2=None, op0=ALU.mult),
         reads=[T_cst], writes=[T_cst])
    D_wp = S.dsem("wpool")
    for l in range(2):
        S.dma("pool", wpool_sb[:, l, :].rearrange("p (g c d) -> p g c d", g=4, c=2),
              w_pool[l].rearrange("g (c p) d -> p g c d", p=128), D_wp, writes=[T_cst])
    for l in range(2):
        S.op("dve", lambda e, l=l: e.memset(st32[l][:], 0.0), writes=[T_st32[l]])
        S.op("dve", lambda e, l=l: e.memset(stb[l][:], 0.0), writes=[T_stb[l]])
        S.op("dve", lambda e, l=l: e.memset(chist[l][:], 0.0), writes=[T_chist[l]])
        S.op("dve", lambda e, l=l: e.memset(phist[l][:], 0.0), writes=[T_phist[l]])

    tiles, blocks = seq_tiles()

    def load_lnt(slot, idx):
        S.dma("sp", lnt[:, slot, :], lnt_d[idx], D_lnt[slot], writes=[T_lnt[slot]])

    def layer_norm_inplace(j, rows, pre_fn=None):
        h = hres[0:rows, j, :]
        tk = T_hres[j]
        stats, t_s = A([128, 2, 6])
        mv, _ = A([128, 2], tok=t_s)
        rstd, _ = A([128, 1], tok=t_s)
        nb, _ = A([128, 1], tok=t_s)
        for c in range(2):
            S.op("dve", lambda e, c=c: e.bn_stats(out=stats[0:rows, c, :], in_=h[:, c * 512:(c + 1) * 512]),
                 reads=[tk], writes=[t_s])
        S.op("dve", lambda e: e.bn_aggr(out=mv[0:rows, :], in_=stats[0:rows, :, :].rearrange("p a b -> p (a b)")), reads=[t_s], writes=[t_s])
        S.op("act", lambda e: e.activation(out=rstd[0:rows, :], in_=mv[0:rows, 1:2], func=AF.Ln, bias=epsc[0:rows, 0:1], scale=1.0),
             reads=[t_s, T_cst], writes=[t_s])
        S.op("act", lambda e: e.activation(out=rstd[0:rows, :], in_=rstd[0:rows, :], func=AF.Exp, scale=-0.5),
             reads=[t_s], writes=[t_s])
        S.op("dve", lambda e: e.scalar_tensor_tensor(out=nb[0:rows, :], in0=mv[0:rows, 0:1], scalar=-1.0,
                                                     in1=rstd[0:rows, :], op0=ALU.mult, op1=ALU.mult),
             reads=[t_s], writes=[t_s])
        S.op("act", lambda e: e.activation(out=h, in_=h, func=AF.Identity, bias=nb[0:rows, :], scale=rstd[0:rows, :]),
             reads=[tk, t_s], writes=[tk])
        S.op("dve", lambda e: e.tensor_tensor(out=h, in0=h, in1=lnt[0:rows, 0, :], op=ALU.mult),
             reads=[tk, T_lnt[0]], writes=[tk])
        S.op("dve", lambda e: e.tensor_tensor(out=h, in0=h, in1=lnt[0:rows, 1, :], op=ALU.add),
             reads=[tk, T_lnt[1]], writes=[tk])

    def transpose_to_hT(j, rows, btok, router, mask0):
        tk = T_hres[j]
        if mask0:
            S.op("dve", lambda e: e.tensor_scalar(out=hres[0:rows, j, :], in0=hres[0:rows, j, :], scalar1=m0,
                                                  scalar2=None, op0=ALU.mult), reads=[tk, T_cst], writes=[tk])
        hb, t_hb = A([128, D], BF16)
        S.op("act", lambda e: e.activation(out=hb[0:rows, :], in_=hres[0:rows, j, :], func=AF.Copy), reads=[tk], writes=[t_hb])
        ps, tp = PS()
        psb = ps[:, :].bitcast(BF16)
        for kc in range(8):
            S.op("pe", lambda e, kc=kc, psb=psb: e.transpose(out=psb[:, kc * 128:kc * 128 + rows],
                                                             in_=hb[0:rows, kc * 128:(kc + 1) * 128],
                                                             identity=identb[0:rows, 0:rows]),
                 reads=[t_hb, T_cst], writes=[tp])
        S.op("act", lambda e, psb=psb: e.activation(out=hT[:, :, btok:btok + rows],
                                                    in_=psb[:, :].rearrange("p (a b) -> p a b", a=8)[:, :, 0:rows], func=AF.Copy),
             reads=[tp], writes=[T_hT[j]])
        if not router:
            return
        hlo, t_hlo = A([128, D], BF16)
        hTlo, t_hTlo = A([128, 8, 128], BF16)
        S.op("dve", lambda e: e.tensor_tensor(out=hlo[0:rows, :], in0=hres[0:rows, j, :], in1=hb[0:rows, :], op=ALU.subtract),
             reads=[tk, t_hb], writes=[t_hlo])
        ps, tp = PS()
        psb = ps[:, :].bitcast(BF16)
        for kc in range(8):
            S.op("pe", lambda e, kc=kc, psb=psb: e.transpose(out=psb[:, kc * 128:kc * 128 + rows],
                                                             in_=hlo[0:rows, kc * 128:(kc + 1) * 128],
                                                             identity=identb[0:rows, 0:rows]),
                 reads=[t_hlo, T_cst], writes=[tp])
        S.op("dve", lambda e, psb=psb: e.tensor_copy(out=hTlo[:, :, 0:rows],
                                                     in_=psb[:, :].rearrange("p (a b) -> p a b", a=8)[:, :, 0:rows]),
             reads=[tp], writes=[t_hTlo])
        ps, tp = PS()
        n = 0
        for kc in range(8):
            for (lt, rd, rh) in ((hT[:, kc, btok:btok + rows], T_hT[j], wrh[:, kc, :]),
                                 (hT[:, kc, btok:btok + rows], T_hT[j], wrl[:, kc, :]),
                                 (hTlo[:, kc, 0:rows], t_hTlo, wrh[:, kc, :])):
                S.op("pe", lambda e, lt=lt, rh=rh, n=n, ps=ps: e.matmul(ps[0:rows, 0:16], lhsT=lt, rhs=rh, start=(n == 0), stop=(n == 23)),
                     reads=[rd, T_cst], writes=[tp])
                n += 1
        R = slice(0, rows)
        mx, t_k = A([128, 1])
        ee, _ = A([128, 4, 4], tok=t_k)
        m1, _ = A([128, 4], tok=t_k)
        eq1, _ = A([128, 4, 4], tok=t_k)
        e2, _ = A([128, 4, 4], tok=t_k)
        m2, _ = A([128, 4], tok=t_k)
        eq2, _ = A([128, 4, 4], tok=t_k)
        gs, _ = A([128, 4], tok=t_k)
        gm, _ = A([128, 1], tok=t_k)
        sr, _ = A([128, 4], tok=t_k)
        w1, _ = A([128, 4], tok=t_k)
        w2, _ = A([128, 4], tok=t_k)
        cb = comb[R, j, :].rearrange("p (g e) -> p g e", g=4)
        tc_ = T_comb[j]
        V = lambda fn, rd=(), wr_=(): S.op("dve", fn, reads=[t_k] + list(rd), writes=[t_k] + list(wr_))
        V(lambda e: e.tensor_reduce(out=mx[R, :], in_=ps[R, 0:16], axis=AX.X, op=ALU.max), rd=[tp])
        V(lambda e: e.tensor_scalar(out=mx[R, :], in0=mx[R, :], scalar1=-1.0, scalar2=None, op0=ALU.mult))
        S.op("act", lambda e: e.activation(out=ee[R].rearrange("p g e -> p (g e)"), in_=ps[R, 0:16], func=AF.Exp,
                                           bias=mx[R, :], scale=1.0), reads=[tp, t_k], writes=[t_k])
        V(lambda e: e.tensor_reduce(out=m1[R, :], in_=ee[R], axis=AX.X, op=ALU.max))
        V(lambda e: e.tensor_tensor(out=eq1[R], in0=ee[R], in1=bc(m1[R, :], 2, [rows, 4, 4]),
                                    op=ALU.is_equal))
        V(lambda e: e.scalar_tensor_tensor(out=e2[R], in0=eq1[R], scalar=-4.0, in1=ee[R], op0=ALU.mult, op1=ALU.add))
        V(lambda e: e.tensor_reduce(out=m2[R, :], in_=e2[R], axis=AX.X, op=ALU.max))
        V(lambda e: e.tensor_tensor(out=eq2[R], in0=e2[R], in1=bc(m2[R, :], 2, [rows, 4, 4]),
                                    op=ALU.is_equal))
        V(lambda e: e.tensor_tensor(out=gs[R, :], in0=m1[R, :], in1=m2[R, :], op=ALU.add))
        V(lambda e: e.tensor_reduce(out=gm[R, :], in_=gs[R, :], axis=AX.X, op=ALU.max))
        V(lambda e: e.tensor_scalar(out=sr[R, :], in0=gs[R, :], scalar1=gm[R, :], scalar2=None, op0=ALU.is_equal))
        V(lambda e: e.reciprocal(out=gm[R, :], in_=gm[R, :]))
        V(lambda e: e.tensor_scalar(out=sr[R, :], in0=sr[R, :], scalar1=gm[R, :], scalar2=None, op0=ALU.mult))
        V(lambda e: e.tensor_tensor(out=w1[R, :], in0=m1[R, :], in1=sr[R, :], op=ALU.mult))
        V(lambda e: e.tensor_tensor(out=w2[R, :], in0=m2[R, :], in1=sr[R, :], op=ALU.mult))
        V(lambda e: e.tensor_tensor(out=eq1[R], in0=eq1[R], in1=bc(w1[R, :], 2, [rows, 4, 4]), op=ALU.mult))
        V(lambda e: e.tensor_tensor(out=eq2[R], in0=eq2[R], in1=bc(w2[R, :], 2, [rows, 4, 4]), op=ALU.mult))
        V(lambda e: e.tensor_tensor(out=cb, in0=eq1[R], in1=eq2[R], op=ALU.add), wr_=[tc_])

    def mixer(l, ti, subs, btok0, first_tile):
        slot0, T = tiles[ti]
        nch = T // CH
        tsl = slice(btok0, btok0 + T)
        T_hTt = [T_hT[j] for j, _, _ in subs]
        cw = convw[:, l]

        A_reset()
        BT, t_BT = A([128, 8, TT], BF16)
        CT, t_CT = A([128, 8, TT], BF16)
        pre = [A([128, 3 + TT]) for _ in range(3)]
        cacc = [A([128, TT]) for _ in range(3)]
        wv_dt, tw_dt = w_in_piece(l, COL_DT, 32)
        dtT, t_dt = A([32, TT])
        dAT, _ = A([32, TT], tok=t_dt)
        acsT, _ = A([32, TT], tok=t_dt)
        sdtT, _ = A([32, TT], tok=t_dt)
        tmpT, _ = A([32, TT], tok=t_dt)
        onesT, _ = A([32, CH], tok=t_dt)
        hiT, t_hl = A([32, TT], BF16)
        loT, _ = A([32, TT], BF16, tok=t_hl)

        def dt_chain():
            ps, tp = PS()
            for kc in range(8):
                S.op("pe", lambda e, kc=kc: e.matmul(ps[0:32, 0:T], lhsT=wv_dt[:, kc, 0:32], rhs=hT[:, kc, tsl],
                                                     start=(kc == 0), stop=(kc == 7)), reads=[tw_dt] + T_hTt, writes=[tp])
            yield
            S.op("act", lambda e: e.activation(out=tmpT[:, 0:T], in_=ps[0:32, 0:T], func=AF.Exp,
                                               bias=vecs[0:32, l, 56:57], scale=1.0), reads=[tp, T_cst], writes=[t_dt])
            S.op("act", lambda e: e.activation(out=dtT[:, 0:T], in_=tmpT[:, 0:T], func=AF.Ln, bias=epsc[0:32, 2:3], scale=1.0),
                 reads=[t_dt], writes=[t_dt])
            yield
            if ti == 0:
                S.op("dve", lambda e: e.memset(dtT[:, 0:48], 0.0), reads=[t_dt], writes=[t_dt])
            S.op("dve", lambda e: e.tensor_scalar(out=dAT[:, 0:T], in0=dtT[:, 0:T], scalar1=Aneg[:, l:l + 1], scalar2=None,
                                                  op0=ALU.mult), reads=[t_dt, T_cst], writes=[t_dt])
            S.op("dve", lambda e: e.memset(onesT[:], 1.0), writes=[t_dt])
            for c in range(nch):
                cs = slice(c * CH, (c + 1) * CH)
                S.op("dve", lambda e, cs=cs: e.tensor_tensor_scan(out=acsT[:, cs], data0=onesT[:], data1=dAT[:, cs],
                                                                  initial=0.0, op0=ALU.mult, op1=ALU.add),
                     reads=[t_dt], writes=[t_dt])
            yield
            for c in range(nch):
                cs = slice(c * CH, (c + 1) * CH)
                S.op("act", lambda e, cs=cs, c=c: e.activation(out=tmpT[:, cs], in_=acsT[:, cs], func=AF.Exp,
                                                               bias=acsT[:, c * CH + 63:c * CH + 64], scale=-1.0),
                     reads=[t_dt], writes=[t_dt])
            S.op("act", lambda e: e.activation(out=hiT[:, 0:T], in_=acsT[:, 0:T], func=AF.Copy), reads=[t_dt], writes=[t_hl])
            yield
            S.op("dve", lambda e: e.tensor_tensor(out=sdtT[:, 0:T], in0=dtT[:, 0:T], in1=tmpT[:, 0:T], op=ALU.mult),
                 reads=[t_dt], writes=[t_dt])
            S.op("dve", lambda e: e.tensor_tensor(out=loT[:, 0:T], in0=acsT[:, 0:T], in1=hiT[:, 0:T], op=ALU.subtract),
                 reads=[t_dt, t_hl], writes=[t_hl])
            yield

        dtg = dt_chain()
        dt_steps = {1: 1, 5: 1, 9: 1, 13: 1, 17: 1} if T == TT else {}
        def hist_in(b):
            pbn, t_pbn = pre[b % 3]
            S.op("dve", lambda e: e.tensor_copy(out=pbn[:, 0:3], in_=chist[l][:, b, :]),
                 reads=[T_chist[l]], writes=[t_pbn])

        hist_in(0)
        pend_silu = None
        for pi in range(8):
            wv, tw = w_in_piece(l, COL_X + pi * 512, 512, idx=pi)
            for cbk in range(4):
                b = pi * 4 + cbk
                ps, tp = PS()
                for kc in range(8):
                    S.op("pe", lambda e, kc=kc, cbk=cbk: e.matmul(ps[:, 0:T], lhsT=wv[:, kc, cbk * 128:(cbk + 1) * 128],
                                                                   rhs=hT[:, kc, tsl], start=(kc == 0), stop=(kc == 7)),
                         reads=[tw] + T_hTt, writes=[tp])
                pb, t_pb = pre[b % 3]
                ca, t_ca = cacc[b % 3]
                S.op("act", lambda e: e.activation(out=pb[:, 3:3 + T], in_=ps[:, 0:T], func=AF.Copy),
                     reads=[tp], writes=[t_pb])
                S.op("dve", lambda e, b=b: e.tensor_copy(out=chist[l][:, b, :], in_=pb[:, T:T + 3]),
                     reads=[t_pb], writes=[T_chist[l]])
                S.op("act", lambda e, b=b: e.activation(out=ca[:, 0:T], in_=pb[:, 0:T], func=AF.Identity,
                                                        bias=cw[:, b, 4:5], scale=cw[:, b, 0:1]),
                     reads=[t_pb, T_cst], writes=[t_ca])
                if pend_silu is not None:
                    pend_silu()
                if b + 1 < 32:
                    hist_in(b + 1)
                for jj in range(1, 4):
                    S.op("dve", lambda e, b=b, jj=jj: e.scalar_tensor_tensor(out=ca[:, 0:T], in0=pb[:, jj:jj + T],
                                                                             scalar=cw[:, b, jj:jj + 1], in1=ca[:, 0:T],
                                                                             op0=ALU.mult, op1=ALU.add),
                         reads=[t_pb, t_ca, T_cst], writes=[t_ca])
                if b < 16:
                    dst, t_dst = xT[:, b, 0:T], T_xT
                elif b < 24:
                    dst, t_dst = BT[:, b - 16, 0:T], t_BT
                else:
                    dst, t_dst = CT[:, b - 24, 0:T], t_CT

                def pend_silu(dst=dst, t_dst=t_dst, ca=ca, t_ca=t_ca):
                    S.op("act", lambda e: e.activation(out=dst, in_=ca[:, 0:T], func=AF.Silu),
                         reads=[t_ca], writes=[t_dst])
                for _ in range(dt_steps.get(b, 0)):
                    next(dtg, None)
        pend_silu()
        for _ in dtg:
            pass

        arow, t_arow = A([2, 2048], BF16)
        tok4, t_tok = A([64, 4, 32])
        nhi, t_n = A([64, 32], BF16)
        nlo, _ = A([64, 32], BF16, tok=t_n)
        eab = [A([64, 32]) for _ in range(2)]
        cdb = [A([128, 32]) for _ in range(2)]
        xs, t_xs = A([64, 32, 64], BF16)
        xsd, t_xsd = A([64, 32, 64], BF16)
        Btok, t_Btok = A([64, 8, 128], BF16)
        Ebb = [A([64, 16, 64], BF16) for _ in range(2)]
        MTb = [A([64, 16, 64], BF16) for _ in range(2)]
        t1, t_t1 = A([64, 16, 64])
        ybf, t_ybf = A([64, 16, 64], BF16)
        cbs, t_cbs = A([64, 8, 64])

        def stage_P(c):
            cs = slice(c * CH, (c + 1) * CH)
            ea, t_ea = eab[c % 2]
            cd, t_cd = cdb[c % 2]
            S.dma("sp", arow[0:1, :], hiT[:, cs], D_row, reads=[t_hl], writes=[t_arow])
            S.dma("sp", arow[1:2, :], loT[:, cs], D_row, reads=[t_hl], writes=[t_arow])
            ps, tp = PS()
            for q, src in enumerate((dtT, acsT, sdtT, dAT)):
                S.op("pe", lambda e, q=q, src=src, ps=ps: e.transpose(out=ps[0:64, q * 32:(q + 1) * 32], in_=src[:, cs],
                                                                      identity=ident32[0:32, 0:32]),
                     reads=[t_dt, T_cst], writes=[tp])
            S.op("act", lambda e, ps=ps: e.activation(out=tok4[:].rearrange("p a b -> p (a b)"), in_=ps[0:64, 0:128], func=AF.Copy),
                 reads=[tp], writes=[t_tok])
            S.op("act", lambda e: e.activation(out=ea[:], in_=tok4[:, 1, :], func=AF.Exp), reads=[t_tok], writes=[t_ea])
            S.op("dve", lambda e: e.tensor_scalar(out=nhi[:], in0=tok4[:, 1, :], scalar1=-1.0, scalar2=None, op0=ALU.mult),
                 reads=[t_tok], writes=[t_n])
            S.op("dve", lambda e: e.scalar_tensor_tensor(out=nlo[:], in0=tok4[:, 1, :], scalar=-1.0, in1=nhi[:],
                                                         op0=ALU.mult, op1=ALU.subtract), reads=[t_tok], writes=[t_n])
            ps2, tp2 = PS()
            S.op("pe", lambda e: e.matmul(ps2[:, 0:32], lhsT=ones32[0:64, :], rhs=tok4[:, 3, :], start=True, stop=True),
                 reads=[t_tok, T_cst], writes=[tp2])
            S.op("act", lambda e: e.activation(out=cd[:], in_=ps2[:, 0:32], func=AF.Exp), reads=[tp2], writes=[t_cd])

        def stage_X(c):
            cs = slice(c * CH, (c + 1) * CH)
            for hh in range(2):
                ps, tp = PS()
                psb = ps[:, :].bitcast(BF16)
                for bb in range(8):
                    b = hh * 8 + bb
                    S.op("pe", lambda e, b=b, bb=bb, psb=psb: e.transpose(out=psb[0:64, bb * 128:(bb + 1) * 128],
                                                                          in_=xT[:, b, cs], identity=identb[:, :]),
                         reads=[T_xT, T_cst], writes=[tp])
                pv = psb[0:64, :].rearrange("p (h d) -> p h d", h=16)
                hs = slice(hh * 16, hh * 16 + 16)
                S.op("dve", lambda e, pv=pv, hs=hs: e.tensor_tensor(out=xs[:, hs, :], in0=pv,
                                                                    in1=bc(tok4[:, 0, hs], 2, [64, 16, 64]),
                                                                    op=ALU.mult), reads=[tp, t_tok], writes=[t_xs])
                S.op("dve", lambda e, pv=pv, hs=hs: e.tensor_tensor(out=xsd[:, hs, :], in0=pv,
                                                                    in1=bc(tok4[:, 2, hs], 2, [64, 16, 64]),
                                                                    op=ALU.mult), reads=[tp, t_tok], writes=[t_xsd])
            ps, tp = PS()
            psb = ps[:, :].bitcast(BF16)
            for g in range(8):
                S.op("pe", lambda e, g=g, psb=psb: e.transpose(out=psb[0:64, g * 128:(g + 1) * 128], in_=BT[:, g, cs],
                                                               identity=identb[:, :]), reads=[t_BT, T_cst], writes=[tp])
            S.op("act", lambda e, psb=psb: e.activation(out=Btok[:].rearrange("p g n -> p (g n)"), in_=psb[0:64, :],
                                                        func=AF.Copy), reads=[tp], writes=[t_Btok])
            psCB, tCB = PS()
            for g in range(8):
                S.op("pe", lambda e, g=g, psCB=psCB: e.matmul(psCB[0:64, g * 64:(g + 1) * 64], lhsT=BT[:, g, cs], rhs=CT[:, g, cs],
                                                              start=True, stop=True), reads=[t_BT, t_CT], writes=[tCB])
            S.op("act", lambda e, psCB=psCB: e.activation(out=cbs[:].rearrange("p g l -> p (g l)"), in_=psCB[0:64, :], func=AF.Copy),
                 reads=[tCB], writes=[t_cbs])

        def stage_S(c):
            for hh in range(2):
                Eb, t_E = Ebb[hh]
                MT, t_MT = MTb[hh]
                for bk in range(2):
                    ps, tp = PS()
                    h0 = hh * 16 + bk * 8
                    outv = ps[0:64, :].rearrange("p (h l) -> p h l", h=8)
                    mm = [
                        (onesb[0:2, 0:64], arow[0:2, h0 * 64:(h0 + 8) * 64].rearrange("p (h l) -> p h l", h=8), [t_arow]),
                        (identb[0:64, 0:64], bc(nhi[:, h0:h0 + 8], 2, [64, 8, 64]), [t_n]),
                        (identb[0:64, 0:64], bc(nlo[:, h0:h0 + 8], 2, [64, 8, 64]), [t_n]),
                        (identb[0:64, 0:64], bc(negmb[:, :], 1, [64, 8, 64]), []),
                    ]
                    for mi, (lt, rh, rd) in enumerate(mm):
                        S.op("pe", lambda e, lt=lt, rh=rh, mi=mi, outv=outv: e.matmul(outv, lhsT=lt, rhs=rh,
                                                                                       start=(mi == 0), stop=(mi == 3)),
                             reads=[T_cst] + rd, writes=[tp])
                    S.op("act", lambda e, ps=ps, bk=bk, Eb=Eb: e.activation(
                        out=Eb[:, bk * 8:(bk + 1) * 8, :].rearrange("p h l -> p (h l)"), in_=ps[0:64, :], func=AF.Exp),
                         reads=[tp], writes=[t_E])
                g0 = hh * 4
                S.op("dve", lambda e, g0=g0, MT=MT, Eb=Eb: e.tensor_tensor(
                    out=MT[:].rearrange("p (g r) l -> p g r l", g=4),
                    in0=Eb[:].rearrange("p (g r) l -> p g r l", g=4),
                    in1=bc(cbs[:, g0:g0 + 4, :], 2, [64, 4, 4, 64]), op=ALU.mult),
                     reads=[t_E, t_cbs], writes=[t_MT])

        def stage_Y(c):
            cs = slice(c * CH, (c + 1) * CH)
            ea, t_ea = eab[c % 2]
            psO, psY = {}, {}
            for hh in range(2):
                psO[hh] = [PS(), PS()]
                for gl in range(4):
                    g = hh * 4 + gl
                    pO, tO = psO[hh][gl // 2]
                    S.op("pe", lambda e, pO=pO, gl=gl, g=g: e.matmul(pO[0:64, (gl % 2) * 256:(gl % 2 + 1) * 256],
                                                                     lhsT=CT[:, g, cs], rhs=stb[l][:, g * 256:(g + 1) * 256],
                                                                     start=True, stop=True),
                         reads=[t_CT, T_stb[l]], writes=[tO])
            for hh in range(2):
                MT, t_MT = MTb[hh]
                psY[hh] = [PS(), PS()]
                for hl in range(16):
                    h = hh * 16 + hl
                    pY, tY = psY[hh][hl // 8]
                    S.op("pe", lambda e, pY=pY, hl=hl, h=h, MT=MT: e.matmul(pY[0:64, (hl % 8) * 64:(hl % 8 + 1) * 64],
                                                                            lhsT=MT[:, hl, :], rhs=xs[:, h, :], start=True, stop=True),
                         reads=[t_MT, t_xs], writes=[tY])
            for hh in range(2):
                for bk in range(2):
                    h0 = hh * 16 + bk * 8
                    pO, tO = psO[hh][bk]
                    pY, tY = psY[hh][bk]
                    S.op("dve", lambda e, pO=pO, bk=bk, h0=h0: e.tensor_tensor(
                        out=t1[:, bk * 8:(bk + 1) * 8, :], in0=pO[0:64, :].rearrange("p (h d) -> p h d", h=8),
                        in1=bc(ea[:, h0:h0 + 8], 2, [64, 8, 64]), op=ALU.mult),
                         reads=[tO, t_ea], writes=[t_t1])
                    S.op("dve", lambda e, pY=pY, bk=bk: e.tensor_tensor(
                        out=ybf[:, bk * 8:(bk + 1) * 8, :], in0=pY[0:64, :].rearrange("p (h d) -> p h d", h=8),
                        in1=t1[:, bk * 8:(bk + 1) * 8, :], op=ALU.add), reads=[tY, t_t1], writes=[t_ybf])
                ps, tp = PS()
                psb = ps[:, :].bitcast(BF16)
                for bb in range(8):
                    S.op("pe", lambda e, bb=bb, psb=psb: e.transpose(out=psb[:, bb * 64:(bb + 1) * 64],
                                                                     in_=ybf[:, 2 * bb:2 * bb + 2, :].rearrange("p h d -> p (h d)"),
                                                                     identity=identb[0:64, 0:64]), reads=[t_ybf, T_cst], writes=[tp])
                S.op("act", lambda e, psb=psb, hh=hh: e.activation(out=yT[:, hh * 8:hh * 8 + 8, cs],
                                                                   in_=psb[:, 0:512].rearrange("p (b l) -> p b l", b=8),
                                                                   func=AF.Copy), reads=[tp], writes=[T_yT])

        def stage_U(c):
            cd, t_cd = cdb[c % 2]
            psS = {}
            for hh in range(2):
                psS[hh] = [PS(), PS()]
                for gl in range(4):
                    g = hh * 4 + gl
                    pS, tS = psS[hh][gl // 2]
                    S.op("pe", lambda e, pS=pS, gl=gl, g=g: e.matmul(pS[:, (gl % 2) * 256:(gl % 2 + 1) * 256],
                                                                     lhsT=Btok[:, g, :],
                                                                     rhs=xsd[:, 4 * g:4 * g + 4, :].rearrange("p h d -> p (h d)"),
                                                                     start=True, stop=True),
                         reads=[t_Btok, t_xsd], writes=[tS])
            for hh in range(2):
                for bk in range(2):
                    h0 = hh * 16 + bk * 8
                    pS, tS = psS[hh][bk]
                    sv = st32[l][:, h0 * 64:(h0 + 8) * 64].rearrange("p (h d) -> p h d", h=8)
                    S.op("dve", lambda e, sv=sv, h0=h0: e.tensor_tensor(out=sv, in0=sv,
                                                                        in1=bc(cd[:, h0:h0 + 8], 2, [128, 8, 64]),
                                                                        op=ALU.mult), reads=[t_cd, T_st32[l]], writes=[T_st32[l]])
                    S.op("dve", lambda e, sv=sv, pS=pS: e.tensor_tensor(out=sv, in0=sv,
                                                                        in1=pS[:, :].rearrange("p (h d) -> p h d", h=8),
                                                                        op=ALU.add), reads=[tS, T_st32[l]], writes=[T_st32[l]])
            S.op("act", lambda e: e.activation(out=stb[l][:], in_=st32[l][:], func=AF.Copy),
                 reads=[T_st32[l]], writes=[T_stb[l]])

        stage_P(0)
        for c in range(nch):
            stage_X(c)
            stage_S(c)
            if c + 1 < nch:
                stage_P(c + 1)
            stage_Y(c)
            stage_U(c)

        if upto == "p1" and dbg:
            dump(tok4[:].rearrange("p a b -> p (a b)"), t_tok, 64, 128)
            dump(st32[l][:, 0:512], T_st32[l], 128, 512)
            dump16(yT[:, :, 0:64], T_yT, 128, 1024)
            dump16(xT[:, :, 0:64], T_xT, 128, 1024)
            raise _Stop()
        A_reset()
        szb = [A([128, TT]) for _ in range(4)]
        ydb = [A([128, TT]) for _ in range(4)]
        sqb = [A([128, TT], BF16) for _ in range(4)]
        vnw = vecs[:, l, 0:16]
        vdsk = vecs[:, l, 16:32]
        vpsc = vecs[:, l, 32:40]
        vbg = vecs[:, l, 40:56]
        ms_all, t_ms = A([128, 8, TT])
        pend_post = [None]

        def z_post(b, sq, t_sq, psn, tn):
            S.op("act", lambda e: e.activation(out=sq[:, 0:T], in_=yT[:, b, 0:T], func=AF.Square),
                 reads=[T_yT], writes=[t_sq])
            S.op("pe", lambda e: e.matmul(psn[:, 0:T], lhsT=onesb[:, :], rhs=sq[:, 0:T],
                                          start=(b % 2 == 0), stop=(b % 2 == 1)),
                 reads=[t_sq, T_cst], writes=[tn])
            if b % 2 == 1:
                S.op("dve", lambda e: e.tensor_scalar(out=ms_all[:, b // 2, 0:T], in0=psn[:, 0:T], scalar1=1.0 / 256.0,
                                                      scalar2=RMS_EPS, op0=ALU.mult, op1=ALU.add),
                     reads=[tn], writes=[t_ms])

        psn_cur = None
        for pi in range(4):
            wv, tw = w_in_piece(l, COL_Z + pi * 512, 512, idx=8 + pi)
            for cbk in range(4):
                b = pi * 4 + cbk
                if b % 2 == 0:
                    psn_cur = PS()
                ps, tp = PS()
                for kc in range(8):
                    S.op("pe", lambda e, kc=kc, cbk=cbk, ps=ps: e.matmul(ps[:, 0:T], lhsT=wv[:, kc, cbk * 128:(cbk + 1) * 128],
                                                                         rhs=hT[:, kc, tsl], start=(kc == 0), stop=(kc == 7)),
                         reads=[tw] + T_hTt, writes=[tp])
                sz, t_sz = szb[b % 4]
                yd, t_yd = ydb[b % 4]
                sq, t_sq = sqb[b % 4]
                S.op("dve", lambda e, b=b, yd=yd: e.scalar_tensor_tensor(out=yd[:, 0:T], in0=xT[:, b, 0:T],
                                                                         scalar=vdsk[:, b:b + 1], in1=yT[:, b, 0:T],
                                                                         op0=ALU.mult, op1=ALU.add),
                     reads=[T_xT, T_yT, T_cst], writes=[t_yd])
                S.op("act", lambda e, ps=ps, sz=sz: e.activation(out=sz[:, 0:T], in_=ps[:, 0:T], func=AF.Silu),
                     reads=[tp], writes=[t_sz])
                if pend_post[0] is not None:
                    pend_post[0]()
                S.op("dve", lambda e, b=b, yd=yd, sz=sz: e.tensor_tensor(out=yT[:, b, 0:T], in0=yd[:, 0:T], in1=sz[:, 0:T],
                                                                         op=ALU.mult), reads=[t_yd, t_sz], writes=[T_yT])
                pend_post[0] = (lambda b=b, sq=sq, t_sq=t_sq, pc=psn_cur: z_post(b, sq, t_sq, pc[0], pc[1]))
        pend_post[0]()
        msf = ms_all[:, :, :].rearrange("p g t -> p (g t)")
        S.op("act", lambda e: e.activation(out=msf, in_=msf, func=AF.Ln), reads=[t_ms], writes=[t_ms])
        S.op("act", lambda e: e.activation(out=msf, in_=msf, func=AF.Exp, scale=-0.5), reads=[t_ms], writes=[t_ms])
        for b in range(16):
            S.op("dve", lambda e, b=b: e.scalar_tensor_tensor(out=yT[:, b, 0:T], in0=yT[:, b, 0:T],
                                                              scalar=vnw[:, b:b + 1], in1=ms_all[:, b // 2, 0:T],
                                                              op0=ALU.mult, op1=ALU.mult),
                 reads=[T_yT, t_ms, T_cst], writes=[T_yT])
        if upto == "p2a":
            dump16(yT[:, :, 0:64], T_yT, 128, 1024)
            raise _Stop()
        mixp, t_mixp = A([128, 8, TT], BF16)
        ppb = [A([128, 15 + TT]) for _ in range(2)]
        pwa = [A([128, 15 + TT]) for _ in range(2)]
        pwb = [A([128, 15 + TT]) for _ in range(2)]
        for pi in range(2):
            wv, tw = w_in_piece(l, COL_POOL + pi * 512, 512, idx=12 + pi)
            for cbk in range(4):
                b = pi * 4 + cbk
                g = b // 2
                ps, tp = PS()
                for kc in range(8):
                    S.op("pe", lambda e, kc=kc, cbk=cbk, ps=ps: e.matmul(ps[:, 0:T], lhsT=wv[:, kc, cbk * 128:(cbk + 1) * 128],
                                                                         rhs=hT[:, kc, tsl], start=(kc == 0), stop=(kc == 7)),
                         reads=[tw] + T_hTt, writes=[tp])
                pp, t_pp = ppb[b % 2]
                wa, t_wa = pwa[b % 2]
                wb, t_wb = pwb[b % 2]
                S.op("dve", lambda e, b=b, pp=pp: e.tensor_copy(out=pp[:, 0:15], in_=phist[l][:, b, :]),
                     reads=[T_phist[l]], writes=[t_pp])
                S.op("act", lambda e, pp=pp, ps=ps: e.activation(out=pp[:, 15:15 + T], in_=ps[:, 0:T], func=AF.Copy),
                     reads=[tp], writes=[t_pp])
                S.op("dve", lambda e, b=b, pp=pp: e.tensor_copy(out=phist[l][:, b, :], in_=pp[:, T:T + 15]),
                     reads=[t_pp], writes=[T_phist[l]])
                src, t_src = pp, t_pp
                bufs = [(wa, t_wa), (wb, t_wb)]
                for si in range(g + 1):
                    sh = 1 << si
                    dstb, t_dstb = bufs[si % 2]
                    n = 15 + T - sh
                    S.op("dve", lambda e, src=src, dstb=dstb, sh=sh, n=n: e.tensor_tensor(
                        out=dstb[:, sh:sh + n], in0=src[:, sh:sh + n], in1=src[:, 0:n], op=ALU.add),
                         reads=[t_src], writes=[t_dstb])
                    src, t_src = dstb, t_dstb
                if ti == 0:
                    S.op("dve", lambda e, src=src, g=g: e.tensor_tensor(out=src[:, 15:15 + T], in0=src[:, 15:15 + T],
                                                                        in1=rc0[:, g, 0:T], op=ALU.mult),
                         reads=[t_src, T_cst], writes=[t_src])
                    S.op("dve", lambda e, src=src, b=b, pp=pp: e.tensor_tensor(out=mixp[:, b, 0:T], in0=src[:, 15:15 + T],
                                                                               in1=pp[:, 15:15 + T], op=ALU.subtract),
                         reads=[t_src, t_pp], writes=[t_mixp])
                else:
                    S.op("dve", lambda e, src=src, b=b, pp=pp, g=g: e.scalar_tensor_tensor(
                        out=mixp[:, b, 0:T], in0=src[:, 15:15 + T], scalar=1.0 / POOLW[g], in1=pp[:, 15:15 + T],
                        op0=ALU.mult, op1=ALU.subtract), reads=[t_src, t_pp], writes=[t_mixp])
        if upto == "p2b":
            raise _Stop()
        mixT, t_mixT = A([128, 8, TT], BF16)
        g0b = [A([128, TT]) for _ in range(2)]
        g1b = [A([128, TT]) for _ in range(2)]
        wplv = wpool_sb[:, l, :].rearrange("p (g c d) -> p g c d", g=4, c=2)
        twpl = T_cst
        w_in_v = w_in[l].rearrange("(kc p) c -> p kc c", p=128)
        w_so_v = w_so[l].rearrange("(i p) d -> p i d", p=128)
        for db in range(8):
            c0 = COL_GATE + db * 128
            pc, tws = wpiece(l, 14 + db, [
                (lambda r: r[:, 0:1024].rearrange("p (kc c) -> p kc c", kc=8), w_in_v[:, :, c0:c0 + 128]),
                (lambda r: r[:, 1024:2048].rearrange("p (kc c) -> p kc c", kc=8), w_in_v[:, :, c0 + 1024:c0 + 1024 + 128]),
                (lambda r: r[:, 2048:4096].rearrange("p (i d) -> p i d", i=16), w_so_v[:, :, db * 128:(db + 1) * 128]),
            ])
            wg0 = pc[:, 0:1024].rearrange("p (kc c) -> p kc c", kc=8)
            wg1 = pc[:, 1024:2048].rearrange("p (kc c) -> p kc c", kc=8)
            wsv = pc[:, 2048:4096].rearrange("p (i d) -> p i d", i=16)
            g0, t_g0 = g0b[db % 2]
            g1, t_g1 = g1b[db % 2]
            for (wv, gg, t_gg, bcol) in ((wg0, g0, t_g0, db), (wg1, g1, t_g1, 8 + db)):
                ps, tp = PS()
                for kc in range(8):
                    S.op("pe", lambda e, kc=kc, ps=ps, wv=wv: e.matmul(ps[:, 0:T], lhsT=wv[:, kc, :],
                                                                       rhs=hT[:, kc, tsl], start=(kc == 0), stop=(kc == 7)),
                         reads=[tws] + T_hTt, writes=[tp])
                S.op("act", lambda e, ps=ps, gg=gg, bcol=bcol: e.activation(out=gg[:, 0:T], in_=ps[:, 0:T], func=AF.Sigmoid,
                                                                            bias=vbg[:, bcol:bcol + 1], scale=1.0),
                     reads=[tp, T_cst], writes=[t_gg])
            psy, tpy = PS()
            for i in range(16):
                S.op("pe", lambda e, i=i, psy=psy, wsv=wsv: e.matmul(psy[:, 0:T], lhsT=wsv[:, i, :],
                                                                     rhs=yT[:, i, 0:T], start=(i == 0), stop=(i == 15)),
                     reads=[tws, T_yT], writes=[tpy])
            psp, tpp = PS()
            gp = db // 2
            for c2 in range(2):
                S.op("pe", lambda e, c2=c2, psp=psp, gp=gp, db=db: e.matmul(psp[:, 0:T],
                                                                            lhsT=wplv[:, gp, c2, (db % 2) * 128:(db % 2 + 1) * 128],
                                                                            rhs=mixp[:, gp * 2 + c2, 0:T], start=(c2 == 0), stop=(c2 == 1)),
                     reads=[twpl, t_mixp], writes=[tpp])
            S.op("dve", lambda e, g0=g0, psy=psy: e.tensor_tensor(out=g0[:, 0:T], in0=g0[:, 0:T], in1=psy[:, 0:T], op=ALU.mult),
                 reads=[tpy, t_g0], writes=[t_g0])
            S.op("dve", lambda e, g1=g1, psp=psp, db=db: e.scalar_tensor_tensor(out=g1[:, 0:T], in0=psp[:, 0:T],
                                                                                scalar=vpsc[:, db:db + 1], in1=g1[:, 0:T],
                                                                                op0=ALU.mult, op1=ALU.mult),
                 reads=[tpp, t_g1, T_cst], writes=[t_g1])
            S.op("dve", lambda e, g0=g0, g1=g1, db=db: e.tensor_tensor(out=mixT[:, db, 0:T], in0=g0[:, 0:T], in1=g1[:, 0:T], op=ALU.add),
                 reads=[t_g0, t_g1], writes=[t_mixT])
        if upto == "p2c":
            dump16(mixT[:, :, 0:64], t_mixT, 128, 512)
            dump16(mixp[:, :, 0:64], t_mixp, 128, 512)
            for q_ in range(2):
                dump(g0b[q_][0][:, 0:64], g0b[q_][1], 128, 64)
                dump(g1b[q_][0][:, 0:64], g1b[q_][1], 128, 64)
            raise _Stop()
        wo = [wpiece(l, 22 + hf, [(lambda r: r[:, :].rearrange("p (k d) -> p k d", k=8),
                                   w_out[l].rearrange("(k p) d -> p k d", p=128)[:, :, hf * 512:(hf + 1) * 512])]) for hf in range(2)]
        if first_tile:
            load_lnt(0, 2 + 4 * l)
            load_lnt(1, 3 + 4 * l)
        for (j, rows, btok) in subs:
            tl = btok - btok0
            for hf in range(2):
                pc, tk = wo[hf]
                wov = pc[:, :].rearrange("p (k d) -> p k d", k=8)
                ps, tp = PS()
                for k in range(8):
                    S.op("pe", lambda e, k=k, ps=ps, wov=wov: e.matmul(ps[0:rows, :], lhsT=mixT[:, k, tl:tl + rows], rhs=wov[:, k, :],
                                                                       start=(k == 0), stop=(k == 7)),
                         reads=[tk, t_mixT], writes=[tp])
                hv = hres[0:rows, j, hf * 512:(hf + 1) * 512]
                S.op("dve", lambda e, hv=hv, ps=ps: e.scalar_tensor_tensor(out=hv, in0=hv, scalar=ALPHA, in1=ps[0:rows, :],
                                                                           op0=ALU.mult, op1=ALU.add),
                     reads=[tp, T_hres[j]], writes=[T_hres[j]])
            if upto == "p2e":
                continue
            layer_norm_inplace(j, rows)
            if upto == "p2f":
                continue
            transpose_to_hT(j, rows, btok, router=(upto != "p2d"), mask0=False)

    def moe(l, allsubs, ntok):
        A_reset()
        load_lnt(0, 4 + 4 * l)
        load_lnt(1, 5 + 4 * l)
        mt = []
        cur = []
        for s_ in allsubs:
            if cur and (s_[2] + s_[1] - cur[0][2]) > 512:
                mt.append(cur)
                cur = []
            cur.append(s_)
        if cur:
            mt.append(cur)
        hmb = [A([128, 4, 512], BF16) for _ in range(2)]
        sgb = [A([128, 512]) for _ in range(2)]
        for (j, rows, btok) in allsubs:
            S.op("act", lambda e, j=j, rows=rows: e.activation(out=hres[0:rows, j, :], in_=hres[0:rows, j, :], func=AF.Copy,
                                                               scale=ALPHA), reads=[T_hres[j]], writes=[T_hres[j]])
        pend_down = [None]
        it = [0]
        for ex in range(NE):
            wg, twg = wload([(lambda r: r[:, :].rearrange("p (k f) -> p k f", k=8),
                              w_eg[l, ex].rearrange("(k p) f -> p k f", p=128))])
            wu, twu = wload([(lambda r: r[:, :].rearrange("p (k f) -> p k f", k=8),
                              w_eu[l, ex].rearrange("(k p) f -> p k f", p=128))])
            wd, twd = wload([(lambda r: r[:, :].rearrange("p (k d) -> p k d", k=4),
                              w_ed[l, ex].rearrange("(k p) d -> p k d", p=128))])
            wgv = wg[:, :].rearrange("p (k f) -> p k f", k=8)
            wuv = wu[:, :].rearrange("p (k f) -> p k f", k=8)
            wdv = wd[:, :].rearrange("p (k d) -> p k d", k=4)
            for grp in mt:
                b0 = grp[0][2]
                n = grp[-1][2] + grp[-1][1] - b0
                rd_hT = [T_hT[j] for j, _, _ in grp]
                hm, t_hm = hmb[it[0] % 2]
                it[0] += 1
                for fb in range(4):
                    psg, tg = PS()
                    psu, tu = PS()
                    for kc in range(8):
                        S.op("pe", lambda e, kc=kc, fb=fb, psg=psg: e.matmul(psg[:, 0:n], lhsT=wgv[:, kc, fb * 128:(fb + 1) * 128],
                                                                             rhs=hT[:, kc, b0:b0 + n], start=(kc == 0), stop=(kc == 7)),
                             reads=[twg] + rd_hT, writes=[tg])
                    for kc in range(8):
                        S.op("pe", lambda e, kc=kc, fb=fb, psu=psu: e.matmul(psu[:, 0:n], lhsT=wuv[:, kc, fb * 128:(fb + 1) * 128],
                                                                             rhs=hT[:, kc, b0:b0 + n], start=(kc == 0), stop=(kc == 7)),
                             reads=[twu] + rd_hT, writes=[tu])
                    sg, t_sg = sgb[fb % 2]
                    S.op("act", lambda e, psg=psg, sg=sg: e.activation(out=sg[:, 0:n], in_=psg[:, 0:n], func=AF.Silu),
                         reads=[tg], writes=[t_sg])
                    S.op("dve", lambda e, psu=psu, sg=sg, fb=fb: e.tensor_tensor(out=hm[:, fb, 0:n], in0=sg[:, 0:n], in1=psu[:, 0:n],
                                                                                 op=ALU.mult), reads=[tu, t_sg], writes=[t_hm])
                def down(grp=grp, b0=b0, hm=hm, t_hm=t_hm, wdv=wdv, twd=twd, ex=ex):
                    for (j, rows, btok) in grp:
                        tl = btok - b0
                        for hf in range(2):
                            ps, tp = PS()
                            for fb in range(4):
                                S.op("pe", lambda e, fb=fb, ps=ps, tl=tl, rows=rows, hf=hf: e.matmul(
                                    ps[0:rows, :], lhsT=hm[:, fb, tl:tl + rows], rhs=wdv[:, fb, hf * 512:(hf + 1) * 512],
                                    start=(fb == 0), stop=(fb == 3)), reads=[t_hm, twd], writes=[tp])
                            hv = hres[0:rows, j, hf * 512:(hf + 1) * 512]
                            S.op("dve", lambda e, hv=hv, ps=ps, j=j, rows=rows: e.scalar_tensor_tensor(
                                out=hv, in0=ps[0:rows, :], scalar=comb[0:rows, j, ex:ex + 1], in1=hv, op0=ALU.mult, op1=ALU.add),
                                 reads=[tp, T_hres[j], T_comb[j]], writes=[T_hres[j]])

                if pend_down[0] is not None:
                    pend_down[0]()
                pend_down[0] = down
        pend_down[0]()
        for (j, rows, btok) in allsubs:
            layer_norm_inplace(j, rows)
            if l + 1 < nlayers:
                transpose_to_hT(j, rows, btok, router=False, mask0=(rows == 64))

    for bi in range(nblocks):
        A_reset()
        tl_list = blocks[bi]
        allsubs = []
        tile_subs = []
        btok = 0
        j = 0
        for ti in tl_list:
            slot0, T = tiles[ti]
            subs = []
            nsub = max(1, T // 128)
            for s_ in range(nsub):
                rows = min(128, T)
                subs.append((j, rows, btok))
                S.dma("sp", hres[0:rows, j, :], xin[slot0 + s_ * 128: slot0 + s_ * 128 + rows, :], D_x[j], writes=[T_hres[j]])
                btok += rows
                j += 1
            tile_subs.append((ti, subs, subs[0][2]))
            allsubs += subs
        try:
            load_lnt(0, 0)
            load_lnt(1, 1)
            for (j, rows, bt) in allsubs:
                layer_norm_inplace(j, rows)
                transpose_to_hT(j, rows, bt, router=False, mask0=(rows == 64))
            if upto == "ln0":
                raise _Stop()
            for l in range(nlayers):
                for k, (ti, subs, bt0) in enumerate(tile_subs):
                    mixer(l, ti, subs, bt0, first_tile=(k == 0))
                    wsc_ready[l] = True
                    if upto in ("tile0", "p2d", "p2e", "p2f"):
                        raise _Stop()
                if upto == "mixer":
                    raise _Stop()
                moe(l, allsubs, btok)
        except _Stop:
            for (j, rows, bt) in allsubs:
                S.dma("sp", out_d[bt:bt + rows, :], hres[0:rows, j, :], D_out, reads=[T_hres[j]], writes=[T_out])
            break
        for (j, rows, bt) in allsubs:
            if rows == 64:
                continue
            orow = tiles[tl_list[0]][0] + bt - 64
            S.dma("sp", out_d[orow:orow + rows, :], hres[0:rows, j, :], D_out, reads=[T_hres[j]], writes=[T_out])
    S.wait_all("sp", [D_out, D_dbg] + D_ring)
    return nc


def _consts():
    c = np.zeros((128, 1024), np.float32)
    c[:, 0:128] = np.eye(128, dtype=np.float32)
    c[:, 128:256] = 1.0
    t = np.arange(64)
    c[0:64, 256:320] = (t[:, None] <= t[None, :]).astype(np.float32)
    c[0:64, 320:384] = np.where(t[None, :] < t[:, None], -30000.0, 0.0).astype(np.float32)
    rc = np.zeros((4, 64), np.float32)
    for g, w in enumerate(POOLW):
        for s in range(64):
            tt = s - 48
            rc[g, s] = 1.0 / w if tt < 0 else 1.0 / min(tt + 1, w)
    c[:, 384:640] = rc.reshape(1, 256)
    c[0:64, 640] = (t >= 48).astype(np.float32)
    c[:, 641] = LN_EPS
    c[:, 642] = RMS_EPS
    c[:, 643] = 1.0
    return c


_NC_CACHE = {}


def kernel(x, meta_tokens, ln_in_g, ln_in_b, w_router, w_in, conv_w, conv_b, dt_bias, a_log, d_skip, ssd_norm_w,
           w_ssd_out, w_pool, pool_scale, b_gate, w_out, ln1_g, ln1_b, w_exp_gate, w_exp_up, w_exp_down, ln2_g, ln2_b,
           _cfg=None):
    f = lambda a: np.ascontiguousarray(np.asarray(a, dtype=np.float32))
    x = f(x)
    cfg = _cfg or {}
    ncores = cfg.get("ncores", 8)
    key = tuple(sorted(cfg.items()))
    if key not in _NC_CACHE:
        _NC_CACHE[key] = build(**cfg)
    nc = _NC_CACHE[key]
    lnrows = [ln_in_g, ln_in_b]
    for l in range(2):
        lnrows += [ln1_g[l], ln1_b[l], ln2_g[l], ln2_b[l]]
    lnt = np.ascontiguousarray(np.broadcast_to(np.stack([f(r) for r in lnrows])[:, None, :], (10, 128, D)))
    convw = np.zeros((2, 128, 32, 5), np.float32)
    vecs = np.zeros((2, 128, 64), np.float32)
    cw, cb = f(conv_w), f(conv_b)
    for l in range(2):
        convw[l, :, :, 0:4] = cw[l].reshape(4, 32, 128).transpose(2, 1, 0)
        convw[l, :, :, 4] = cb[l].reshape(32, 128).T
        vecs[l, :, 0:16] = f(ssd_norm_w)[l].reshape(16, 128).T
        vecs[l, :, 16:32] = np.repeat(f(d_skip)[l], 64).reshape(16, 128).T
        vecs[l, :, 32:40] = f(pool_scale)[l].reshape(8, 128).T
        vecs[l, :, 40:56] = f(b_gate)[l].reshape(16, 128).T
        vecs[l, 0:32, 56] = f(dt_bias)[l]
        vecs[l, 0:32, 57] = f(a_log)[l]
    wr = np.ascontiguousarray(f(w_router).reshape(8, 128, 16).transpose(1, 0, 2))
    shared = {
        "w_in": f(w_in), "w_so": f(w_ssd_out), "w_pool": f(w_pool), "w_out": f(w_out),
        "w_eg": f(w_exp_gate), "w_eu": f(w_exp_up), "w_ed": f(w_exp_down),
        "lnt": lnt, "cst": _consts(), "convw": convw, "vecs": vecs, "wr": wr,
    }
    in_maps = []
    meta = f(meta_tokens)
    for c in range(ncores):
        b = c % 4
        xin = np.zeros((NSLOT, D), np.float32)
        xin[48:64] = meta
        xin[64:] = x[b]
        m = dict(shared)
        m["xin"] = xin
        in_maps.append(m)
    tr = bool(cfg.get("trace"))
    res = run_bass_kernel_spmd(nc, in_maps, core_ids=list(range(ncores)), **({"trace": True} if tr else {}))
    if tr:
        print("EXEC_TIME_NS", res.exec_time_ns)
    out = np.stack([np.asarray(res.results[b]["out"], dtype=np.float32) for b in range(min(4, ncores))], axis=0)
    if cfg.get("dbg"):
        kernel.last_dbg = [np.asarray(res.results[b]["dbg"]) for b in range(min(4, ncores))]
        kernel.last_dbgb = [np.asarray(res.results[b]["dbgb"]).astype(np.float32) for b in range(min(4, ncores))]
    return out
```
